# Optimizing a Trainium2 kernel written in Bass

```python
import jax, jax.numpy as jnp
from jax import lax
import numpy as np

D_MODEL = 1024
BATCH = 8
SEQ = 4096
DEPTH = 1

ATTN_HEADS = 8
ATTN_WIDTH = D_MODEL // 2
ATTN_HEAD_DIM = ATTN_WIDTH // ATTN_HEADS
MLSTM_HEADS = 4
MLSTM_WIDTH = D_MODEL - ATTN_WIDTH
MLSTM_HEAD_DIM = MLSTM_WIDTH // MLSTM_HEADS
IN_COLS = 3 * ATTN_WIDTH + 2 * MLSTM_WIDTH
CONV_WIDTH = 4
MLSTM_CHUNK = 64
MOBA_BLOCK = 256
MOBA_TOPK = 3
MOBA_QCHUNK = 32
D_FF = ((8 * D_MODEL // 3 + 127) // 128) * 128
N_MOD = 9
EPS = 1e-6

kernel_name = 'hymba_moba_mlstm_macaron_adaln'


def rmsnorm(x, g):
    xf = x.astype(jnp.float32)
    y = xf * lax.rsqrt(jnp.mean(xf * xf, axis=-1, keepdims=True) + EPS)
    return (y * g.astype(jnp.float32)).astype(x.dtype)


def modulate(h, shift, scale):
    return h * (1 + scale) + shift


def swiglu(h, w_gate, w_up, w_down):
    return (jax.nn.silu(h @ w_gate) * (h @ w_up)) @ w_down


def causal_depthwise_conv(x, w, b):
    C = x.shape[-1]
    out = lax.conv_general_dilated(x, w[:, None, :], window_strides=(1,),
                                   padding=((CONV_WIDTH - 1, 0),),
                                   dimension_numbers=('NWC', 'WIO', 'NWC'),
                                   feature_group_count=C)
    return out + b


def moba_attention(q, k, v):
    B, H, S, d = q.shape
    s_pad = -(-S // MOBA_BLOCK) * MOBA_BLOCK
    pad = s_pad - S
    if pad:
        padw = ((0, 0), (0, 0), (0, pad), (0, 0))
        q, k, v = jnp.pad(q, padw), jnp.pad(k, padw), jnp.pad(v, padw)
    nb = s_pad // MOBA_BLOCK
    topk = min(MOBA_TOPK, nb)
    k_blocks = k.reshape(B, H, nb, MOBA_BLOCK, d)
    v_blocks = v.reshape(B, H, nb, MOBA_BLOCK, d)
    k_mean = jnp.mean(k_blocks.astype(jnp.float32), axis=3)
    gate = jnp.einsum('bhsd,bhnd->bhsn', q.astype(jnp.float32), k_mean)
    q_block = jnp.arange(s_pad) // MOBA_BLOCK
    fully_past = jnp.arange(nb)[None, :] < q_block[:, None]
    gate = jnp.where(fully_past, gate, -jnp.inf)
    top_val, top_idx = lax.top_k(gate, topk)
    sel_valid = jnp.isfinite(top_val)

    n_chunks = s_pad // MOBA_QCHUNK

    def to_chunks(a):
        return jnp.moveaxis(a.reshape(B, H, n_chunks, MOBA_QCHUNK, *a.shape[3:]), 2, 0)

    b_idx = jnp.arange(B)[:, None, None]
    h_idx = jnp.arange(H)[None, :, None]
    scale = d ** -0.5
    n_sel = topk * MOBA_BLOCK

    def attend_chunk(args):
        q_c, idx_c, valid_c, ci = args
        blk = ci * MOBA_QCHUNK // MOBA_BLOCK
        flat = idx_c.reshape(B, H, MOBA_QCHUNK * topk)
        k_sel = k_blocks[b_idx, h_idx, flat].reshape(B, H, MOBA_QCHUNK, n_sel, d)
        v_sel = v_blocks[b_idx, h_idx, flat].reshape(B, H, MOBA_QCHUNK, n_sel, d)
        s_sel = jnp.einsum('bhqd,bhqnd->bhqn', q_c, k_sel).astype(jnp.float32) * scale
        s_sel = jnp.where(jnp.repeat(valid_c, MOBA_BLOCK, axis=-1), s_sel, -jnp.inf)
        k_own = lax.dynamic_index_in_dim(k_blocks, blk, axis=2, keepdims=False)
        v_own = lax.dynamic_index_in_dim(v_blocks, blk, axis=2, keepdims=False)
        s_own = jnp.einsum('bhqd,bhkd->bhqk', q_c, k_own).astype(jnp.float32) * scale
        q_pos = ci * MOBA_QCHUNK + jnp.arange(MOBA_QCHUNK)
        k_pos = blk * MOBA_BLOCK + jnp.arange(MOBA_BLOCK)
        s_own = jnp.where(k_pos[None, :] <= q_pos[:, None], s_own, -jnp.inf)
        p = jax.nn.softmax(jnp.concatenate([s_sel, s_own], axis=-1), axis=-1).astype(v.dtype)
        return (jnp.einsum('bhqn,bhqnd->bhqd', p[..., :n_sel], v_sel)
                + jnp.einsum('bhqk,bhkd->bhqd', p[..., n_sel:], v_own))

    out = lax.map(attend_chunk, (to_chunks(q), to_chunks(top_idx), to_chunks(sel_valid),
                                 jnp.arange(n_chunks)))
    out = jnp.moveaxis(out, 0, 2).reshape(B, H, s_pad, d)
    return out[:, :, :S]


def mlstm_chunkwise(q, k, v, i_pre, log_f):
    B, H, S, d = q.shape
    L = MLSTM_CHUNK
    nc = S // L

    def chunks(a):
        return jnp.moveaxis(a.reshape(B, H, nc, L, *a.shape[3:]), 2, 0)

    qc, kc, vc, ic = chunks(q), chunks(k), chunks(v), chunks(i_pre)
    bc = jnp.cumsum(chunks(log_f), axis=-1)
    causal = jnp.tril(jnp.ones((L, L), dtype=bool))

    def step(carry, xs):
        C, n, m = carry
        q_, k_, v_, i_, b_ = xs
        log_d = jnp.where(causal, b_[..., :, None] - b_[..., None, :] + i_[..., None, :], -jnp.inf)
        log_inter = b_ + m[..., None]
        m_t = jnp.maximum(log_inter, jnp.max(log_d, axis=-1))
        w_intra = jnp.exp(log_d - m_t[..., None])
        w_inter = jnp.exp(log_inter - m_t)
        s = jnp.einsum('bhtd,bhsd->bhts', q_, k_) * w_intra
        num = (jnp.einsum('bhts,bhsv->bhtv', s, v_)
               + w_inter[..., None] * jnp.einsum('bhvk,bhtk->bhtv', C, q_))
        den = jnp.sum(s, axis=-1) + w_inter * jnp.einsum('bhk,bhtk->bht', n, q_)
        h = num / jnp.maximum(jnp.abs(den), jnp.exp(-m_t))[..., None]
        b_last = b_[..., -1]
        log_g = b_last[..., None] - b_ + i_
        m_new = jnp.maximum(b_last + m, jnp.max(log_g, axis=-1))
        w_g = jnp.exp(log_g - m_new[..., None])
        decay = jnp.exp(b_last + m - m_new)
        C = decay[..., None, None] * C + jnp.einsum('bhsv,bhsk->bhvk', v_ * w_g[..., None], k_)
        n = decay[..., None] * n + jnp.einsum('bhs,bhsk->bhk', w_g, k_)
        return (C, n, m_new), h

    init = (jnp.zeros((B, H, d, d), jnp.float32), jnp.zeros((B, H, d), jnp.float32),
            jnp.zeros((B, H), jnp.float32))
    _, h = lax.scan(step, init, (qc, kc, vc, ic, bc))
    return jnp.moveaxis(h, 0, 2).reshape(B, H, S, d)


def hybrid_mixer(h, w_in, conv_w, conv_b, w_q_m, w_k_m, w_v_m, w_if, b_if,
                 mlstm_norm, mlstm_skip, attn_norm, w_out):
    B, S, _ = h.shape
    proj = h @ w_in
    q_a, k_a, v_a, x_m, z = jnp.split(
        proj, [ATTN_WIDTH, 2 * ATTN_WIDTH, 3 * ATTN_WIDTH, 3 * ATTN_WIDTH + MLSTM_WIDTH], axis=-1)

    def heads_a(t):
        return t.reshape(B, S, ATTN_HEADS, ATTN_HEAD_DIM).transpose(0, 2, 1, 3)
    o_a = moba_attention(heads_a(q_a), heads_a(k_a), heads_a(v_a))
    o_a = rmsnorm(o_a.transpose(0, 2, 1, 3).reshape(B, S, ATTN_WIDTH), attn_norm)

    x_conv = jax.nn.silu(causal_depthwise_conv(x_m, conv_w, conv_b))
    def headwise(t, w):
        return jnp.einsum('bshd,hde->bshe', t.reshape(B, S, MLSTM_HEADS, MLSTM_HEAD_DIM), w)
    q_m = headwise(x_conv, w_q_m)
    k_m = headwise(x_conv, w_k_m)
    v_m = headwise(x_m, w_v_m)
    gates = (jnp.concatenate([q_m, k_m, v_m], axis=-1).reshape(B, S, 3 * MLSTM_WIDTH) @ w_if
             + b_if).astype(jnp.float32)
    i_pre = gates[..., :MLSTM_HEADS].transpose(0, 2, 1)
    log_f = jax.nn.log_sigmoid(gates[..., MLSTM_HEADS:]).transpose(0, 2, 1)
    to_h = lambda t: t.transpose(0, 2, 1, 3).astype(jnp.float32)
    h_m = mlstm_chunkwise(to_h(q_m), to_h(k_m) * MLSTM_HEAD_DIM ** -0.5, to_h(v_m), i_pre, log_f)
    h_m = h_m.transpose(0, 2, 1, 3)
    mu = jnp.mean(h_m, axis=-1, keepdims=True)
    var = jnp.mean(jnp.square(h_m - mu), axis=-1, keepdims=True)
    h_m = ((h_m - mu) * lax.rsqrt(var + EPS)).reshape(B, S, MLSTM_WIDTH)
    h_m = h_m * mlstm_norm.astype(jnp.float32)
    o_m = (h_m.astype(h.dtype) + mlstm_skip * x_conv) * jax.nn.silu(z)

    return jnp.concatenate([o_a, o_m], axis=-1) @ w_out


def setup_inputs(seed: int = 0) -> dict:
    key = jax.random.key(seed)
    ks = jax.random.split(key, 32)
    L, D, F = DEPTH, D_MODEL, D_FF
    H, W, dh = MLSTM_HEADS, MLSTM_WIDTH, MLSTM_HEAD_DIM

    def nrm(k, shape, scale):
        return jax.random.normal(k, shape, jnp.float32) * scale

    b_if = jnp.concatenate([nrm(ks[15], (L, H), 0.1),
                            jnp.broadcast_to(jnp.linspace(3.0, 6.0, H, dtype=jnp.float32), (L, H))
                            + nrm(ks[16], (L, H), 0.1)], axis=-1)
    return {
        'x': nrm(ks[0], (BATCH, SEQ, D), 1.0),
        'c': nrm(ks[1], (BATCH, D), 1.0),
        'w_ada': nrm(ks[2], (L, D, N_MOD * D), 0.2 * D ** -0.5),
        'b_ada': nrm(ks[3], (L, N_MOD * D), 0.02),
        'ffn1_norm': 1.0 + nrm(ks[4], (L, D), 0.02),
        'ffn1_w_gate': nrm(ks[5], (L, D, F), D ** -0.5),
        'ffn1_w_up': nrm(ks[6], (L, D, F), D ** -0.5),
        'ffn1_w_down': nrm(ks[7], (L, F, D), F ** -0.5),
        'mix_norm': 1.0 + nrm(ks[8], (L, D), 0.02),
        'w_in': nrm(ks[9], (L, D, IN_COLS), D ** -0.5),
        'conv_w': nrm(ks[10], (L, CONV_WIDTH, W), CONV_WIDTH ** -0.5),
        'conv_b': nrm(ks[11], (L, W), 0.02),
        'w_q_m': nrm(ks[12], (L, H, dh, dh), dh ** -0.5),
        'w_k_m': nrm(ks[13], (L, H, dh, dh), dh ** -0.5),
        'w_v_m': nrm(ks[14], (L, H, dh, dh), dh ** -0.5),
        'w_if': nrm(ks[17], (L, 3 * W, 2 * H), 0.1 * (3 * W) ** -0.5),
        'b_if': b_if,
        'mlstm_norm': 1.0 + nrm(ks[18], (L, W), 0.02),
        'mlstm_skip': 1.0 + nrm(ks[19], (L, W), 0.02),
        'attn_norm': 1.0 + nrm(ks[20], (L, ATTN_WIDTH), 0.02),
        'w_out': nrm(ks[21], (L, D, D), D ** -0.5),
        'ffn2_norm': 1.0 + nrm(ks[22], (L, D), 0.02),
        'ffn2_w_gate': nrm(ks[23], (L, D, F), D ** -0.5),
        'ffn2_w_up': nrm(ks[24], (L, D, F), D ** -0.5),
        'ffn2_w_down': nrm(ks[25], (L, F, D), F ** -0.5),
        'final_norm': 1.0 + nrm(ks[26], (D,), 0.02),
    }


def reference(x, c, w_ada, b_ada, ffn1_norm, ffn1_w_gate, ffn1_w_up, ffn1_w_down,
              mix_norm, w_in, conv_w, conv_b, w_q_m, w_k_m, w_v_m, w_if, b_if,
              mlstm_norm, mlstm_skip, attn_norm, w_out,
              ffn2_norm, ffn2_w_gate, ffn2_w_up, ffn2_w_down, final_norm):
    B = x.shape[0]
    for l in range(DEPTH):
        mod = (c @ w_ada[l] + b_ada[l]).reshape(B, N_MOD, D_MODEL)
        sh1, sc1, g1, sh2, sc2, g2, sh3, sc3, g3 = [mod[:, None, i] for i in range(N_MOD)]
        h = modulate(rmsnorm(x, ffn1_norm[l]), sh1, sc1)
        x = x + 0.5 * (1 + g1) * swiglu(h, ffn1_w_gate[l], ffn1_w_up[l], ffn1_w_down[l])
        h = modulate(rmsnorm(x, mix_norm[l]), sh2, sc2)
        x = x + (1 + g2) * hybrid_mixer(h, w_in[l], conv_w[l], conv_b[l], w_q_m[l], w_k_m[l],
                                        w_v_m[l], w_if[l], b_if[l], mlstm_norm[l], mlstm_skip[l],
                                        attn_norm[l], w_out[l])
        h = modulate(rmsnorm(x, ffn2_norm[l]), sh3, sc3)
        x = x + 0.5 * (1 + g3) * swiglu(h, ffn2_w_gate[l], ffn2_w_up[l], ffn2_w_down[l])
    return rmsnorm(x, final_norm)
```

```python
from contextlib import ExitStack
import numpy as np
import concourse.bass as bass
import concourse.mybir as mybir
from concourse.bass_utils import run_bass_kernel_spmd

F32 = mybir.dt.float32
BF16 = mybir.dt.bfloat16
ALU = mybir.AluOpType
AF = mybir.ActivationFunctionType
AX = mybir.AxisListType

D = 1024
FF = 2816
NFF = 22
TT = 512
EPS = 1e-6
NR = 6
BIG = 30000.0


class Sched:
    COMPUTE = ("pe", "act", "dve", "pool")

    def __init__(self, nc, self_sync=True):
        self.nc = nc
        self.self_sync = self_sync
        self.eng = {"pe": nc.tensor, "act": nc.scalar, "dve": nc.vector, "pool": nc.gpsimd, "sp": nc.sync}
        self.prog = {k: [] for k in self.eng}
        self.cnt = {k: 0 for k in self.COMPUTE}
        self.dcnt = {}
        self.waited = {k: {} for k in self.eng}
        self.acc = {}
        self.semkeys = set(self.COMPUTE)
        self.sems = {}

    @staticmethod
    def box(ap):
        t = ap.tensor
        name = t.name
        dims = list(ap.ap)
        esz = 2 if ap.dtype == BF16 else 4
        if type(t).__name__.startswith("DRam"):
            lo = ap.offset
            hi = lo + sum((c - 1) * abs(s) for s, c in dims) + 1
            return (name, 0, 1, lo * esz, hi * esz)
        pstride = dims[0][0]
        pcnt = dims[0][1]
        if pstride == 0:
            p0, f0 = 0, ap.offset
        else:
            p0 = ap.offset // pstride
            f0 = ap.offset - p0 * pstride
        ext = sum((c - 1) * abs(s) for s, c in dims[1:]) + 1
        lo, hi = f0 * esz, (f0 + ext) * esz
        if type(t).__name__.startswith("PSum"):
            return (name, 0, 128, (lo // 2048) * 2048, -(-hi // 2048) * 2048)
        return (name, p0, p0 + pcnt, lo, hi)

    @staticmethod
    def _ov(a, b):
        return a[1] < b[2] and b[1] < a[2] and a[3] < b[4] and b[3] < a[4]

    @staticmethod
    def _contains(a, b):
        return a[1] <= b[1] and b[2] <= a[2] and a[3] <= b[3] and b[4] <= a[4]

    def _deps(self, reads, writes):
        deps = []
        for b in reads:
            for rec in self.acc.get(b[0], ()):
                if rec[0] == "W" and self._ov(rec[1], b):
                    deps.append(rec[2])
        for b in writes:
            for rec in self.acc.get(b[0], ()):
                if self._ov(rec[1], b):
                    deps.append(rec[2])
        return deps

    def _record(self, reads, writes, comp):
        for b in writes:
            lst = self.acc.setdefault(b[0], [])
            lst[:] = [r for r in lst if not self._contains(b, r[1])]
            lst.append(("W", b, comp))
        for b in reads:
            lst = self.acc.setdefault(b[0], [])
            lst[:] = [r for r in lst if not (r[0] == "R" and r[2][0] == comp[0] and r[2][1] <= comp[1]
                                             and self._contains(b, r[1]))]
            lst.append(("R", b, comp))

    def _emit_waits(self, e, deps):
        best = {}
        for k, v in deps:
            if v > best.get(k, 0):
                best[k] = v
        for k, v in best.items():
            if k == e and (e == "pe" or not self.self_sync):
                continue
            if k == e and v > self.cnt[e]:
                continue
            if self.waited[e].get(k, 0) >= v:
                continue
            self.waited[e][k] = v
            self.prog[e].append(("wait", k, v))

    def op(self, e, fn, reads=(), writes=(), inc=True):
        rb = [self.box(a) for a in reads]
        wb = [self.box(a) for a in writes]
        self._emit_waits(e, self._deps(rb, wb))
        comp = (e, self.cnt[e] + 1)
        self.prog[e].append(("op", fn, e if inc else None))
        self._record(rb, wb, comp)
        if inc:
            self.cnt[e] += 1
        return comp

    def dma(self, q, out, in_, key, **kw):
        rb = [self.box(in_)]
        wb = [self.box(out)]
        self._emit_waits(q, self._deps(rb, wb))
        self.semkeys.add(key)
        n = self.dcnt.get(key, 0) + 1
        self.dcnt[key] = n
        comp = (key, 16 * n)

        def fn(eng, out=out, in_=in_, kw=kw):
            return eng.dma_start(out=out, in_=in_, **kw)
        self.prog[q].append(("dma", fn, key))
        self._record(rb, wb, comp)
        return comp

    def final_wait(self, e="sp"):
        deps = [(k, 16 * n) for k, n in self.dcnt.items()]
        deps += [(k, self.cnt[k]) for k in self.COMPUTE if self.cnt[k] > 0 and k != e]
        self._emit_waits(e, deps)

    def emit(self, es):
        nc = self.nc
        for k in sorted(self.semkeys, key=str):
            self.sems[k] = es.enter_context(nc.semaphore("s_" + str(k)))
        block = es.enter_context(nc.Block())

        def make(e):
            def body(eng):
                for item in self.prog[e]:
                    if item[0] == "wait":
                        eng.wait_ge(self.sems[item[1]], item[2])
                    elif item[0] == "op":
                        ins = item[1](eng)
                        if item[2] is not None:
                            ins.then_inc(self.sems[item[2]], 1)
                    else:
                        item[1](eng).then_inc(self.sems[item[2]], 16)
            return body
        for e, reg in (("sp", block.sync), ("act", block.scalar), ("dve", block.vector),
                       ("pool", block.gpsimd), ("pe", block.tensor)):
            if self.prog[e]:
                reg(make(e))


class _Stop(Exception):
    pass


def build(S_LEN, dbg=False, stage=99):
    NT = S_LEN // TT
    NKT = S_LEN // 128
    NB = S_LEN // 256
    nc = bass.Bass("TRN2", target_bir_lowering=False)

    def din(name, shape):
        return nc.dram_tensor(name, shape, F32, kind="ExternalInput").ap()

    x = din("x", [S_LEN, D])
    vecs = din("vecs", [72, 128])
    bada = din("bada", [72, 128])
    w_ada = din("w_ada", [D, 9 * D])
    f1g, f1u, f1d = din("f1g", [D, FF]), din("f1u", [D, FF]), din("f1d", [FF, D])
    f2g, f2u, f2d = din("f2g", [D, FF]), din("f2u", [D, FF]), din("f2d", [FF, D])
    w_in = din("w_in", [D, 2560])
    w_out = din("w_out", [D, D])
    wqkv = din("wqkv", [3, 4, 128, 128])
    w_if = din("w_if", [1536, 8])
    b_if = din("b_if", [8])
    y = nc.dram_tensor("y", [S_LEN, D], F32, kind="ExternalOutput").ap()

    es = ExitStack()
    with es:
        S = Sched(nc)

        def sb(name, shape, dt=F32):
            return es.enter_context(nc.sbuf_tensor(name, shape, dt))

        ident_f = sb("ident_f", [128, 128])
        ones_m = sb("ones_m", [128, 128], BF16)
        ones_f = sb("ones_f", [128, 128])
        tri_f = sb("tri_f", [128, 128])
        causal4 = sb("causal4", [128, 4, 512], BF16)
        ind16 = sb("ind16", [80, 16, 128], BF16)
        E01 = sb("E01", [128, 32])
        ELB = sb("ELB", [128, 32])
        OWN = sb("OWN", [128, 32])
        vecT = sb("vecT", [128, 72])
        modT = sb("modT", [128, 72])
        prm = sb("prm", [128, 9, 8])
        modrow = sb("modrow", [1, 2, 256])
        wqkv_b = sb("wqkv_b", [128, 12, 128], BF16)
        wif_sb = sb("wif_sb", [128, 12, 8])
        wg_b = sb("wg_b", [128, 8, 8], BF16)
        bif_bc = sb("bif_bc", [128, 8])
        Kc = sb("Kc", [128, 4, S_LEN], BF16)
        Vc = sb("Vc", [128, NKT, 8, 65], BF16)
        km_f = sb("km_f", [128, 4, 16])
        km_hi = sb("km_hi", [128, 4, 16], BF16)
        km_lo = sb("km_lo", [128, 4, 16], BF16)
        Cst = sb("Cst", [128, 4, 129])
        Cb = sb("Cb", [128, 4, 129], BF16)
        xm_halo = sb("xm_halo", [128, 4, 3], BF16)
        xT = sb("xT", [128, 8, TT])
        hT = sb("hT", [128, 8, TT], BF16)
        tmpf = sb("tmpf", [128, 2, TT])
        rstd = sb("rstd", [128, 2, TT])
        ring = sb("ring", [128, NR * 2048], BF16)
        UB = 60 * 1024
        U = sb("U", [128, UB // 2], BF16)
        Uf = U.bitcast(F32)
        ringf = ring.bitcast(F32)
        stg = ringf[:, 0:1536].rearrange("p (a e) -> p a e", a=12)
        wqkvT = ringf[:, 1536:3072].rearrange("p (a e) -> p a e", a=12)
        PS = [es.enter_context(nc.psum_tensor(f"ps{i}", [128, 512], F32)) for i in range(8)]

        class Carver:
            def __init__(self):
                self.off = 0

            def b(self, n):
                o = self.off
                self.off += -(-n * 2 // 64) * 64
                assert self.off <= UB, self.off
                return U[:, o // 2:o // 2 + n]

            def f(self, n):
                o = self.off
                self.off += -(-n * 4 // 64) * 64
                assert self.off <= UB, self.off
                return Uf[:, o // 4:o // 4 + n]

        cv = Carver()
        act = cv.b(NFF * TT).rearrange("p (j t) -> p j t", j=NFF)
        x_tm = cv.f(4 * D).rearrange("p (s d) -> p s d", s=4)
        cv_mark = cv.off
        cv.off = 0
        sq = cv.b(8 * TT).rearrange("p (c t) -> p c t", c=8)
        cv.off = 0
        Qn = cv.b(4 * TT).rearrange("p (c t) -> p c t", c=4)
        xm_b = cv.b(4 * 515).rearrange("p (c t) -> p c t", c=4)
        sz = cv.b(4 * TT).rearrange("p (c t) -> p c t", c=4)
        xc_b = cv.b(4 * TT).rearrange("p (c t) -> p c t", c=4)
        xcs = cv.b(4 * TT).rearrange("p (c t) -> p c t", c=4)
        PT = cv.b(3 * TT).rearrange("p (c t) -> p c t", c=3)
        biasT = cv.b(2 * TT).rearrange("p (c t) -> p c t", c=2)
        biasq = cv.f(4 * 128).rearrange("p (s h k) -> p s h k", s=4, h=8)
        gm = cv.f(128).rearrange("p (h k) -> p h k", h=8)
        sel = cv.f(128).rearrange("p (h k) -> p h k", h=8)
        thr = cv.f(64).rearrange("p (h k) -> p h k", h=8)
        convacc = cv.f(TT)
        att_f = cv.f(4 * TT).rearrange("p (c t) -> p c t", c=4)
        o_sb = cv.f(TT)
        lr_f = biasT.rearrange("p c t -> p (c t)").bitcast(F32)
        QtS = [cv.b(512).rearrange("p (h t) -> p h t", h=4)] * 2
        KtS = [cv.b(512).rearrange("p (h t) -> p h t", h=4)] * 2
        Ktok = [cv.b(512).rearrange("p (h t) -> p h t", h=4)] * 2
        Vp = [cv.b(4 * 129).rearrange("p (h t) -> p h t", h=4)] * 2
        Sc = [cv.b(512).rearrange("p (h t) -> p h t", h=4)] * 2
        hn = [cv.f(512).rearrange("p (h t) -> p h t", h=4)] * 2
        gsm = [cv.f(64)] * 2
        bst = cv.f(4 * 6).rearrange("p (h k) -> p h k", h=4)
        mv = cv.f(8).rearrange("p (h k) -> p h k", h=4)
        ctmp = cv.f(4 * 129).rearrange("p (h t) -> p h t", h=4)
        o_cat = cv.b(8 * TT).rearrange("p (c t) -> p c t", c=8)
        wa_st = x_tm.rearrange("p s (k n) -> p (s k) n", n=256)
        wa_bufs = [wa_st[:, 0:8, :], wa_st[:, 8:16, :]]

        def mm(out, lhsT, rhs, start, stop, inc=None):
            inc = stop if inc is None else inc
            S.op("pe", lambda e: e.matmul(out, lhsT=lhsT, rhs=rhs, start=start, stop=stop),
                 reads=[lhsT, rhs], writes=[out], inc=inc)

        def tr(out, in_, ident, inc=True):
            S.op("pe", lambda e: e.transpose(out=out, in_=in_, identity=ident),
                 reads=[in_, ident], writes=[out], inc=inc)

        def actf(out, in_, func, bias=0.0, scale=1.0, extra_reads=()):
            S.op("act", lambda e: e.activation(out=out, in_=in_, func=func, bias=bias, scale=scale),
                 reads=[in_] + list(extra_reads), writes=[out])

        def acopy(out, in_):
            S.op("act", lambda e: e.copy(out=out, in_=in_), reads=[in_], writes=[out])

        def vcopy(out, in_):
            S.op("dve", lambda e: e.tensor_copy(out=out, in_=in_), reads=[in_], writes=[out])

        def vtt(out, in0, in1, op):
            S.op("dve", lambda e: e.tensor_tensor(out=out, in0=in0, in1=in1, op=op), reads=[in0, in1], writes=[out])

        def vts(out, in0, s1, s2, op0, op1=None, rd=()):
            if op1 is None:
                S.op("dve", lambda e: e.tensor_scalar(out=out, in0=in0, scalar1=s1, scalar2=None, op0=op0),
                     reads=[in0] + list(rd), writes=[out])
            else:
                S.op("dve", lambda e: e.tensor_scalar(out=out, in0=in0, scalar1=s1, scalar2=s2, op0=op0, op1=op1),
                     reads=[in0] + list(rd), writes=[out])

        def vstt(out, in0, scalar, in1, op0, op1, rd=()):
            S.op("dve", lambda e: e.scalar_tensor_tensor(out=out, in0=in0, scalar=scalar, in1=in1, op0=op0, op1=op1),
                 reads=[in0, in1] + list(rd), writes=[out])

        def pmemset(ap, v):
            S.op("pool", lambda e: e.memset(ap, v), writes=[ap])

        def vmemset(ap, v):
            S.op("dve", lambda e: e.memset(ap, v), writes=[ap])

        def pasel(ap, pattern, cmp, cm, base=0):
            S.op("pool", lambda e: e.affine_select(out=ap, in_=ap, pattern=pattern, compare_op=cmp, fill=0.0,
                                                   base=base, channel_multiplier=cm), reads=[ap], writes=[ap])

        dumps = {}

        def chk(st):
            if stage <= st:
                raise _Stop()

        def dump(name, ap, dt=F32):
            if not dbg or name in dumps:
                return
            shp = list(ap.shape)
            dumps[name] = nc.dram_tensor("dbg_" + name, shp, dt, kind="ExternalOutput").ap()
            S.dma("sp", dumps[name], ap, "dbg_" + name)

        pmemset(ident_f[:], 1.0)
        pasel(ident_f[:], [[-1, 128]], ALU.is_equal, 1)
        pmemset(ones_m[:], 1.0 / D)
        pmemset(ones_f[:], 1.0)
        pmemset(tri_f[:], 1.0)
        pasel(tri_f[:], [[1, 128]], ALU.is_ge, -1)
        pmemset(causal4[:], 1.0)
        pasel(causal4[:], [[-128, 4], [1, 512]], ALU.is_ge, -1)
        pmemset(ind16[:], 1.0)
        pasel(ind16[0:16], [[1, 16], [0, 128]], ALU.is_equal, -1)
        pasel(ind16[64:80], [[1, 16], [0, 128]], ALU.is_equal, -1)
        pmemset(E01[:, 0:16], 1.0)
        pmemset(E01[:, 16:32], 0.0)
        pmemset(ELB[:, 0:16], 0.0)
        pmemset(ELB[:, 16:32], -1e30)
        pmemset(OWN[:], 0.0)
        pmemset(OWN[:, 16:17], 1.0)
        pmemset(Vc[:, :, :, 64:65], 1.0)
        pmemset(km_f[:], 0.0)
        pmemset(km_hi[:], 0.0)
        pmemset(km_lo[:], 0.0)
        pmemset(Cst[:], 0.0)
        pmemset(Cb[:], 0.0)
        pmemset(xm_halo[:], 0.0)

        S.dma("sp", stg[0:72, 0, :], vecs, "ldv1")
        tr(PS[0][:, 0:72], stg[0:72, 0, :], ident_f[0:72, 0:72])
        vcopy(vecT[:], PS[0][:, 0:72])
        S.dma("sp", stg[0:72, 1, :], bada, "ldv2")
        tr(PS[1][:, 0:72], stg[0:72, 1, :], ident_f[0:72, 0:72])
        vcopy(modT[:], PS[1][:, 0:72])
        S.dma("sp", bif_bc[:], b_if.partition_broadcast(128), "ldv3")
        S.dma("sp", wif_sb[:], w_if.rearrange("(j p) g -> p j g", p=128), "ldv4")

        NG = 36
        for g in range(NG):
            buf = wa_bufs[g % 2]
            S.dma("sp", buf, w_ada[:, g * 256:(g + 1) * 256].rearrange("(kc p) n -> p kc n", p=128), f"lda{g % 2}")
            pso = PS[2 + g % 2]
            for kc in range(8):
                mm(pso[0:1, 0:256], vecT[:, kc:kc + 1], buf[:, kc, :], kc == 0, kc == 7)
            acopy(modrow[0:1, g % 2, :], pso[0:1, 0:256])
            for jj in range(2):
                j = 2 * g + jj
                mm(PS[4][:, j:j + 1], modrow[0:1, g % 2, jj * 128:(jj + 1) * 128], ones_f[0:1, 0:1], True, True)
        vtt(modT[:], modT[:], PS[4][:, 0:72], ALU.add)

        def modv(i):
            return modT[:, i * 8:(i + 1) * 8]

        def vrow(r, n=8):
            return vecT[:, r:r + n]
        for blk, (nrow, half) in enumerate(((8, 0.5), (16, 1.0), (24, 0.5))):
            vstt(prm[:, 3 * blk + 0, :], modv(3 * blk + 1), 1.0, vrow(nrow), ALU.add, ALU.mult)
            vcopy(prm[:, 3 * blk + 1, :], modv(3 * blk + 0))
            vts(prm[:, 3 * blk + 2, :], modv(3 * blk + 2), 1.0, half, ALU.add, ALU.mult)
        fin_g = vrow(32)
        attn_g = vrow(40, 4)
        mn_g = vrow(44, 4)
        skip_g = vrow(48, 4)
        conv_bv = vrow(52, 4)

        def conv_wv(j, hc):
            return vecT[:, 56 + j * 4 + hc:57 + j * 4 + hc]

        S.dma("sp", stg[:], wqkv.rearrange("a h d e -> d (a h) e"), "ldv5")
        vcopy(wqkv_b[:], stg[:])
        for i in range(12):
            tr(PS[5 + i % 2][:, 0:128], stg[:, i, :], ident_f[:])
            acopy(wqkvT[:, i, :], PS[5 + i % 2][:, 0:128])
        for h in range(4):
            mm(PS[7][:, h * 8:(h + 1) * 8], wqkvT[:, h, :], wif_sb[:, 3 * h, :], True, False)
            mm(PS[7][:, h * 8:(h + 1) * 8], wqkvT[:, 4 + h, :], wif_sb[:, 3 * h + 1, :], False, True, inc=False)
            mm(PS[7][:, (4 + h) * 8:(5 + h) * 8], wqkvT[:, 8 + h, :], wif_sb[:, 3 * h + 2, :], True, True, inc=(h == 3))
        vcopy(wg_b[:].rearrange("p a g -> p (a g)"), PS[7][:, 0:64])

        units = []

        def add_unit_cols(w, c0):
            units.append(w[:, c0:c0 + 256].rearrange("(kc p) n -> p kc n", p=128))

        def add_unit_rows(w, r0):
            units.append(w[r0:r0 + 256, :].rearrange("(jj p) n -> p jj n", p=128))

        for t in range(NT):
            for g in range(11):
                add_unit_cols(f1g, g * 256)
                add_unit_cols(f1u, g * 256)
            for g in range(11):
                add_unit_rows(f1d, g * 256)
            for g in range(10):
                add_unit_cols(w_in, g * 256)
            for g in range(4):
                add_unit_cols(w_out, g * 256)
            for g in range(11):
                add_unit_cols(f2g, g * 256)
                add_unit_cols(f2u, g * 256)
            for g in range(11):
                add_unit_rows(f2d, g * 256)
        wstate = {"issued": 0, "next": 0}

        def wview(u, rows):
            slot = ring[:, (u % NR) * 2048:(u % NR + 1) * 2048]
            if rows:
                return slot.rearrange("p (jj n) -> p jj n", jj=2)
            return slot.rearrange("p (kc n) -> p kc n", kc=8)

        def wissue_upto(n):
            while wstate["issued"] < min(n, len(units)):
                u = wstate["issued"]
                src = units[u]
                rows = (src.shape[1] == 2)
                S.dma("pool", wview(u, rows), src, f"w{u % NR}")
                wstate["issued"] += 1

        def wnext(rows=False):
            u = wstate["next"]
            wstate["next"] += 1
            wissue_upto(u + NR - 1)
            return wview(u, rows)

        def rms_mod(ns, sh):
            for c in range(8):
                actf(sq[:, c, :], xT[:, c, :], AF.Square)
            for c in range(8):
                mm(PS[6][:], ones_m[:], sq[:, c, :], c == 0, c == 7)
            actf(rstd[:, 0, :], PS[6][:], AF.Sqrt, bias=eps_t[:, 0:1], scale=1.0, extra_reads=[eps_t[:, 0:1]])
            S.op("dve", lambda e: e.reciprocal(out=rstd[:, 1, :], in_=rstd[:, 0, :]),
                 reads=[rstd[:, 0, :]], writes=[rstd[:, 1, :]])
            for c in range(8):
                vstt(tmpf[:, c % 2, :], xT[:, c, :], ns[:, c:c + 1], rstd[:, 1, :], ALU.mult, ALU.mult,
                     rd=[ns[:, c:c + 1]])
                actf(hT[:, c, :], tmpf[:, c % 2, :], AF.Identity, bias=sh[:, c:c + 1], scale=1.0,
                     extra_reads=[sh[:, c:c + 1]])

        def ffn(rg):
            for g in range(11):
                wg_v = wnext()
                wu_v = wnext()
                for jj in range(2):
                    j = 2 * g + jj
                    pg, pu = PS[j % 2], PS[2 + j % 2]
                    for kc in range(8):
                        mm(pg[:], wg_v[:, kc, jj * 128:(jj + 1) * 128], hT[:, kc, :], kc == 0, kc == 7)
                    for kc in range(8):
                        mm(pu[:], wu_v[:, kc, jj * 128:(jj + 1) * 128], hT[:, kc, :], kc == 0, kc == 7)
                    actf(tmpf[:, j % 2, :], pg[:], AF.Silu)
                    vtt(act[:, j, :], tmpf[:, j % 2, :], pu[:], ALU.mult)
            dump('act', act[:], BF16)
            for g in range(11):
                wd_v = wnext(rows=True)
                for jj in range(2):
                    j = 2 * g + jj
                    for i in range(8):
                        mm(PS[i][:], wd_v[:, jj, i * 128:(i + 1) * 128], act[:, j, :], j == 0, j == NFF - 1,
                           inc=(i == 7 and jj == 1))
            for i in range(8):
                vstt(xT[:, i, :], PS[i][:], rg[:, i:i + 1], xT[:, i, :], ALU.mult, ALU.add, rd=[rg[:, i:i + 1]])

        eps_t = sb("eps_t", [128, 1])
        pmemset(eps_t[:], EPS)

        def mixer(t):
            c0 = t * TT
            for g in range(4):
                wv_ = wnext()
                for jj in range(2):
                    oc = 2 * g + jj
                    ps = PS[4 + oc % 2]
                    for kc in range(8):
                        mm(ps[:], wv_[:, kc, jj * 128:(jj + 1) * 128], hT[:, kc, :], kc == 0, kc == 7)
                    if oc < 4:
                        acopy(Qn[:, oc, :], ps[:])
                    else:
                        vcopy(Kc[:, oc - 4, c0:c0 + TT], ps[:])
            for g in range(2):
                wv_ = wnext()
                for s4 in range(4):
                    ps = PS[6 + s4 % 2]
                    for kc in range(8):
                        mm(ps[:, 0:256], hT[:, kc, s4 * 128:(s4 + 1) * 128], wv_[:, kc, :], kc == 0, kc == 7)
                    dst = Vc[:, t * 4 + s4, g * 4:(g + 1) * 4, 0:64]
                    src = ps[:, 0:256].rearrange("p (h e) -> p h e", h=4)
                    if s4 % 2 == 0:
                        acopy(dst, src)
                    else:
                        vcopy(dst, src)
            vcopy(xm_b[:, :, 0:3], xm_halo[:])
            for g in range(2):
                wv_ = wnext()
                for jj in range(2):
                    oc = 2 * g + jj
                    ps = PS[4 + oc % 2]
                    for kc in range(8):
                        mm(ps[:], wv_[:, kc, jj * 128:(jj + 1) * 128], hT[:, kc, :], kc == 0, kc == 7)
                    acopy(xm_b[:, oc, 3:515], ps[:])
            vcopy(xm_halo[:], xm_b[:, :, 512:515])
            for g in range(2):
                wv_ = wnext()
                for jj in range(2):
                    oc = 2 * g + jj
                    ps = PS[6 + oc % 2]
                    for kc in range(8):
                        mm(ps[:], wv_[:, kc, jj * 128:(jj + 1) * 128], hT[:, kc, :], kc == 0, kc == 7)
                    actf(sz[:, oc, :], ps[:], AF.Silu)
            for hc in range(4):
                vts(convacc, xm_b[:, hc, 0:512], conv_wv(0, hc), conv_bv[:, hc:hc + 1], ALU.mult, ALU.add,
                    rd=[conv_wv(0, hc), conv_bv[:, hc:hc + 1]])
                for j in range(1, 4):
                    vstt(convacc, xm_b[:, hc, j:j + 512], conv_wv(j, hc), convacc, ALU.mult, ALU.add,
                         rd=[conv_wv(j, hc)])
                actf(xc_b[:, hc, :], convacc, AF.Silu)
                vts(xcs[:, hc, :], xc_b[:, hc, :], skip_g[:, hc:hc + 1], None, ALU.mult, rd=[skip_g[:, hc:hc + 1]])
            dump('Qn', Qn[:], BF16); dump('xm_b', xm_b[:], BF16); dump('sz', sz[:], BF16); dump('xc_b', xc_b[:], BF16)
            chk(3)
            attention(t)
            chk(4)
            dump('att_f', att_f[:]); dump('biasq', biasq[:])
            mlstm(t)
            chk(5)
            dump('o_cat', o_cat[:], BF16)
            rg = prm[:, 5, :]
            for g in range(4):
                wv_ = wnext()
                for jj in range(2):
                    oc = 2 * g + jj
                    ps = PS[4 + oc % 2]
                    for kc in range(8):
                        mm(ps[:], wv_[:, kc, jj * 128:(jj + 1) * 128], o_cat[:, kc, :], kc == 0, kc == 7)
                    vstt(xT[:, oc, :], ps[:], rg[:, oc:oc + 1], xT[:, oc, :], ALU.mult, ALU.add, rd=[rg[:, oc:oc + 1]])

        def attention(t):
            c0 = t * TT
            for c in range(4):
                for b in range(2):
                    blk = 2 * t + b
                    src = Kc[:, c, blk * 256:(blk + 1) * 256]
                    S.op("dve", lambda e, src=src, c=c, blk=blk: e.tensor_reduce(
                        out=km_f[:, c, blk:blk + 1], in_=src, axis=AX.X, op=ALU.add),
                        reads=[src], writes=[km_f[:, c, blk:blk + 1]])
                kk = km_f[:, c, 2 * t:2 * t + 2]
                vts(kk, kk, 1.0 / 256, None, ALU.mult)
                vcopy(km_hi[:, c, 2 * t:2 * t + 2], kk)
                vtt(kk, kk, km_hi[:, c, 2 * t:2 * t + 2], ALU.subtract)
                vcopy(km_lo[:, c, 2 * t:2 * t + 2], kk)
            chk(3.1)
            for s4 in range(4):
                qb = 2 * t + s4 // 2
                for h in range(8):
                    c, r0 = h // 2, 64 * (h % 2)
                    psG = PS[6 + h % 2]
                    qsl = Qn[r0:r0 + 64, c, s4 * 128:(s4 + 1) * 128]
                    mm(psG[:, c * 16:(c + 1) * 16], qsl, km_hi[r0:r0 + 64, c, :], True, False)
                    mm(psG[:, c * 16:(c + 1) * 16], qsl, km_lo[r0:r0 + 64, c, :], False, True, inc=(h >= 6))
                chk(3.11)
                elb = ELB[:, 16 - qb:32 - qb].unsqueeze(1).broadcast_to([128, 8, 16])
                e01 = E01[:, 16 - qb:32 - qb].unsqueeze(1).broadcast_to([128, 8, 16])
                own = OWN[:, 16 - qb:32 - qb].unsqueeze(1).broadcast_to([128, 8, 16])
                gm4 = gm.rearrange("p (c two) k -> p c two k", two=2)
                for par in range(2):
                    vtt(gm4[:, :, par, :], PS[6 + par][:, 0:64].rearrange("p (c k) -> p c k", c=4),
                        ELB[:, 16 - qb:32 - qb].unsqueeze(1).broadcast_to([128, 4, 16]), ALU.add)
                chk(3.12)
                for h in range(8):
                    S.op("dve", lambda e, h=h: e.max(out=thr[:, h, :], in_=gm[:, h, :]),
                         reads=[gm[:, h, :]], writes=[thr[:, h, :]])
                chk(3.13)
                vtt(sel, gm, thr[:, :, 2:3].broadcast_to([128, 8, 16]), ALU.is_ge)
                chk(3.14)
                vtt(sel, sel, e01, ALU.mult)
                vtt(sel, sel, own, ALU.add)
                vts(biasq[:, s4, :, :], sel, 1.0, BIG, ALU.subtract, ALU.mult)
                chk(3.15)
                if s4 == 1:
                    chk(3.16)
            chk(3.2)
            nkt = 4 * (t + 1)
            for h in range(8):
                c, r0 = h // 2, 64 * (h % 2)
                bT = biasT[:, h % 2, :]
                psT = PS[7]
                for s4 in range(4):
                    tr(psT[0:16, s4 * 128:(s4 + 1) * 128], biasq[:, s4, h, :], ident_f[:], inc=(s4 == 3))
                vcopy(bT[r0:r0 + 16, :], psT[0:16, :])
                chk(3.3)
                pso = PS[3 + h % 2]
                for kt in range(nkt):
                    pss = PS[kt % 3]
                    mm(pss[:], Kc[r0:r0 + 64, c, kt * 128:(kt + 1) * 128], Qn[r0:r0 + 64, c, :], True, False)
                    mm(pss[:], ind16[r0:r0 + 16, kt // 2, :], bT[r0:r0 + 16, :], False, True)
                    pt = PT[:, kt % 3, :]
                    actf(pt, pss[:], AF.Exp, scale=0.125)
                    if kt >= 4 * t:
                        vtt(pt, pt, causal4[:, kt - 4 * t, :], ALU.mult)
                    mm(pso[0:65, :], Vc[:, kt, h, :], pt, kt == 0, kt == nkt - 1, inc=True)
                chk(3.4)
                actf(lr_f[32:33, :], pso[64:65, :], AF.Ln)
                actf(lr_f[64:65, :], lr_f[32:33, :], AF.Exp, scale=-1.0)
                mm(PS[5][0:64, :], ones_f[64:65, 0:64], lr_f[64:65, :], True, True)
                acopy(o_sb[0:64, :], pso[0:64, :])
                vtt(att_f[r0:r0 + 64, c, :], o_sb[0:64, :], PS[5][0:64, :], ALU.mult)
                chk(3.5)
            chk(3.6)
            for c in range(4):
                actf(sq[:, c, :], att_f[:, c, :], AF.Square, scale=2.0 ** 0.5)
            for c in range(4):
                mm(PS[6][:], ones_m[:], sq[:, c, :], c == 0, c == 3)
            actf(rstd[:, 0, :], PS[6][:], AF.Sqrt, bias=eps_t[:, 0:1], scale=1.0, extra_reads=[eps_t[:, 0:1]])
            S.op("dve", lambda e: e.reciprocal(out=rstd[:, 1, :], in_=rstd[:, 0, :]),
                 reads=[rstd[:, 0, :]], writes=[rstd[:, 1, :]])
            for c in range(4):
                vstt(o_cat[:, c, :], att_f[:, c, :], attn_g[:, c:c + 1], rstd[:, 1, :], ALU.mult, ALU.mult,
                     rd=[attn_g[:, c:c + 1]])

        def mlstm(t):
            for ch in range(4):
                cs = slice(ch * 128, (ch + 1) * 128)
                cs3 = slice(3 + ch * 128, 3 + (ch + 1) * 128)
                b2 = ch % 2
                gs = gsm[b2]
                psg = PS[0]
                for h in range(4):
                    mm(psg[:, 0:8], xc_b[:, h, cs], wg_b[:, h, :], h == 0, False)
                for h in range(4):
                    mm(psg[:, 0:8], xm_b[:, h, cs3], wg_b[:, 4 + h, :], False, h == 3)
                vtt(gs[:, 0:8], psg[:, 0:8], bif_bc[:], ALU.add)
                actf(gs[:, 28:32], gs[:, 4:8], AF.Exp, scale=-1.0)
                actf(gs[:, 4:8], gs[:, 28:32], AF.Ln, bias=1.0)
                vts(gs[:, 4:8], gs[:, 4:8], -1.0, None, ALU.mult)
                mm(PS[0][:, 16:20], tri_f[:], gs[:, 4:8], True, True)
                mm(PS[0][:, 24:28], ones_f[:], gs[:, 4:8], True, True)
                vcopy(gs[:, 8:16].rearrange("p (a k) -> p a k", a=2),
                      PS[0][:, 16:32].rearrange("p (a k) -> p a k", a=2)[:, :, 0:4])
                vtt(gs[:, 28:32], gs[:, 0:4], gs[:, 8:12], ALU.subtract)
                actf(gs[:, 16:20], gs[:, 28:32], AF.Exp)
                actf(gs[:, 20:28], gs[:, 8:16], AF.Exp)
                pq, pk, pkt, pvt = PS[1], PS[2], PS[3], PS[4]
                for h in range(4):
                    mm(pq[:, h * 128:(h + 1) * 128], wqkv_b[:, h, :], xc_b[:, h, cs], True, True, inc=(h == 3))
                for h in range(4):
                    mm(pk[:, h * 128:(h + 1) * 128], wqkv_b[:, 4 + h, :], xc_b[:, h, cs], True, True, inc=(h == 3))
                for h in range(4):
                    mm(pkt[:, h * 128:(h + 1) * 128], xc_b[:, h, cs], wqkv_b[:, 4 + h, :], True, True, inc=(h == 3))
                for h in range(4):
                    mm(pvt[:, h * 128:(h + 1) * 128], xm_b[:, h, cs3], wqkv_b[:, 8 + h, :], True, True, inc=(h == 3))
                kscale = 128.0 ** -0.5
                acopy(QtS[b2].rearrange("p h t -> p (h t)"), pq[:])
                actf(KtS[b2].rearrange("p h t -> p (h t)"), pk[:], AF.Copy, scale=kscale)
                actf(Ktok[b2].rearrange("p h t -> p (h t)"), pkt[:], AF.Copy, scale=kscale)
                a_bc = gs[:, 16:20].unsqueeze(2).broadcast_to([128, 4, 128])
                vtt(Vp[b2][:, :, 0:128], pvt[:].rearrange("p (h e) -> p h e", h=4), a_bc, ALU.mult)
                vcopy(Vp[b2][:, :, 128:129], gs[:, 16:20].unsqueeze(2))
                pS = PS[5]
                for h in range(4):
                    mm(pS[:, h * 128:(h + 1) * 128], KtS[b2][:, h, :], QtS[b2][:, h, :], True, True, inc=(h == 3))
                vtt(Sc[b2][:], pS[:].rearrange("p (h t) -> p h t", h=4),
                    tri_f[:].unsqueeze(1).broadcast_to([128, 4, 128]), ALU.mult)
                pH = [PS[6], PS[7]]
                for h in range(4):
                    o = pH[h // 2][:, (h % 2) * 129:(h % 2) * 129 + 129]
                    mm(o, Sc[b2][:, h, :], Vp[b2][:, h, :], True, False)
                    mm(o, QtS[b2][:, h, :], Cb[:, h, :], False, True, inc=(h % 2 == 1))
                pU = [PS[1], PS[2]]
                for h in range(4):
                    o = pU[h // 2][:, (h % 2) * 129:(h % 2) * 129 + 129]
                    mm(o, Ktok[b2][:, h, :], Vp[b2][:, h, :], True, True, inc=(h % 2 == 1))
                for h in range(4):
                    hp, hh = h // 2, h % 2
                    S.op("dve", lambda e, hp=hp, hh=hh, h=h: e.bn_stats(out=bst[:, h, :], in_=pH[hp][:, hh * 129:hh * 129 + 128]),
                         reads=[pH[hp][:, hh * 129:hh * 129 + 128]], writes=[bst[:, h, :]])
                for hp in range(2):
                    Hv = pH[hp][:, 0:258].rearrange("p (h v) -> p h v", h=2)
                    vtt(gs[:, 28 + 2 * hp:30 + 2 * hp].unsqueeze(2), Hv[:, :, 128:129],
                        gs[:, 20 + 2 * hp:22 + 2 * hp].unsqueeze(2), ALU.mult)
                for h in range(4):
                    S.op("dve", lambda e, h=h: e.bn_aggr(out=mv[:, h, :], in_=bst[:, h, :]),
                         reads=[bst[:, h, :]], writes=[mv[:, h, :]])
                vts(gs[:, 52:56], gs[:, 28:32], -1.0, None, ALU.mult)
                vtt(gs[:, 32:36], gs[:, 28:32], gs[:, 52:56], ALU.max)
                vts(gs[:, 32:36], gs[:, 32:36], 1.0, None, ALU.max)
                S.op("dve", lambda e, gs=gs: e.reciprocal(out=gs[:, 36:40], in_=gs[:, 32:36]),
                     reads=[gs[:, 32:36]], writes=[gs[:, 36:40]])
                vtt(gs[:, 36:40], gs[:, 36:40], gs[:, 20:24], ALU.mult)
                vtt(gs[:, 40:44], gs[:, 36:40], gs[:, 36:40], ALU.mult)
                vtt(gs[:, 40:44].unsqueeze(2), gs[:, 40:44].unsqueeze(2), mv[:, :, 1:2], ALU.mult)
                actf(gs[:, 44:48], gs[:, 40:44], AF.Sqrt, bias=eps_t[:, 0:1], scale=1.0, extra_reads=[eps_t[:, 0:1]])
                S.op("dve", lambda e, gs=gs: e.reciprocal(out=gs[:, 48:52], in_=gs[:, 44:48]),
                     reads=[gs[:, 44:48]], writes=[gs[:, 48:52]])
                vtt(gs[:, 48:52], gs[:, 48:52], gs[:, 36:40], ALU.mult)
                for h in range(4):
                    hp, hh = h // 2, h % 2
                    vts(hn[b2][:, h, :], pH[hp][:, hh * 129:hh * 129 + 128], mv[:, h, 0:1], gs[:, 48 + h:49 + h],
                        ALU.subtract, ALU.mult, rd=[mv[:, h, 0:1], gs[:, 48 + h:49 + h]])
                for hp in range(2):
                    vtt(ctmp[:, 2 * hp:2 * hp + 2, :], Cst[:, 2 * hp:2 * hp + 2, :],
                        pU[hp][:, 0:258].rearrange("p (h v) -> p h v", h=2), ALU.add)
                vtt(Cst[:], ctmp[:], gs[:, 24:28].unsqueeze(2).broadcast_to([128, 4, 129]), ALU.mult)
                vcopy(Cb[:], Cst[:])
                pT = PS[3]
                for h in range(4):
                    tr(pT[:, h * 128:(h + 1) * 128], hn[b2][:, h, :], ident_f[:], inc=(h == 3))
                for h in range(4):
                    vstt(tmpf[:, 0, h * 128:(h + 1) * 128], pT[:, h * 128:(h + 1) * 128], mn_g[:, h:h + 1],
                         xcs[:, h, cs], ALU.mult, ALU.add, rd=[mn_g[:, h:h + 1]])
                    vtt(o_cat[:, 4 + h, cs], tmpf[:, 0, h * 128:(h + 1) * 128], sz[:, h, cs], ALU.mult)

        try:
          chk(0)
          for t in range(NT):
              c0 = t * TT
              S.dma("sp", x_tm, x[c0:c0 + TT, :].rearrange("(s p) d -> p s d", p=128), "ldx")
              for c in range(8):
                  ps = PS[6 + c % 2]
                  for s4 in range(4):
                      tr(ps[:, s4 * 128:(s4 + 1) * 128], x_tm[:, s4, c * 128:(c + 1) * 128], ident_f[:], inc=(s4 == 3))
                  acopy(xT[:, c, :], ps[:])
              dump('modT', modT[:]); dump('prm', prm[:]); dump('xT0', xT[:])
              rms_mod(prm[:, 0, :], prm[:, 1, :])
              dump('hT1', hT[:], BF16)
              chk(1)
              ffn(prm[:, 2, :])
              chk(2)
              dump('xT1', xT[:])
              rms_mod(prm[:, 3, :], prm[:, 4, :])
              dump('hT2', hT[:], BF16)
              mixer(t)
              dump('xT2', xT[:])
              rms_mod(prm[:, 6, :], prm[:, 7, :])
              ffn(prm[:, 8, :])
              for c in range(8):
                  actf(sq[:, c, :], xT[:, c, :], AF.Square)
              for c in range(8):
                  mm(PS[6][:], ones_m[:], sq[:, c, :], c == 0, c == 7)
              actf(rstd[:, 0, :], PS[6][:], AF.Sqrt, bias=eps_t[:, 0:1], scale=1.0, extra_reads=[eps_t[:, 0:1]])
              S.op("dve", lambda e: e.reciprocal(out=rstd[:, 1, :], in_=rstd[:, 0, :]),
                   reads=[rstd[:, 0, :]], writes=[rstd[:, 1, :]])
              for c in range(8):
                  vstt(tmpf[:, c % 2, :], xT[:, c, :], fin_g[:, c:c + 1], rstd[:, 1, :], ALU.mult, ALU.mult,
                       rd=[fin_g[:, c:c + 1]])
                  ps = PS[4 + c % 2]
                  for s4 in range(4):
                      tr(ps[:, s4 * 128:(s4 + 1) * 128], tmpf[:, c % 2, s4 * 128:(s4 + 1) * 128], ident_f[:], inc=(s4 == 3))
                  acopy(x_tm[:, :, c * 128:(c + 1) * 128], ps[:].rearrange("p (s d) -> p s d", s=4))
              S.dma("sp", y[c0:c0 + TT, :].rearrange("(s p) d -> p s d", p=128), x_tm, "sty")
        except _Stop:
            pass
        S.final_wait("sp")
        S.emit(es)
    return nc


def make_in_maps(S_LEN, x, c, w_ada, b_ada, ffn1_norm, ffn1_w_gate, ffn1_w_up, ffn1_w_down,
                 mix_norm, w_in, conv_w, conv_b, w_q_m, w_k_m, w_v_m, w_if, b_if,
                 mlstm_norm, mlstm_skip, attn_norm, w_out,
                 ffn2_norm, ffn2_w_gate, ffn2_w_up, ffn2_w_down, final_norm):
    f = lambda a: np.ascontiguousarray(np.asarray(a, dtype=np.float32))
    B = x.shape[0]
    shared = {
        "bada": f(b_ada[0]).reshape(72, 128),
        "w_ada": f(w_ada[0]),
        "f1g": f(ffn1_w_gate[0]), "f1u": f(ffn1_w_up[0]), "f1d": f(ffn1_w_down[0]),
        "f2g": f(ffn2_w_gate[0]), "f2u": f(ffn2_w_up[0]), "f2d": f(ffn2_w_down[0]),
        "w_in": f(w_in[0]), "w_out": f(w_out[0]),
        "wqkv": f(np.stack([w_q_m[0], w_k_m[0], w_v_m[0]])),
        "w_if": f(w_if[0]), "b_if": f(b_if[0]),
    }
    maps = []
    for b in range(B):
        rows = [f(c[b]).reshape(8, 128), f(ffn1_norm[0]).reshape(8, 128), f(mix_norm[0]).reshape(8, 128),
                f(ffn2_norm[0]).reshape(8, 128), f(final_norm).reshape(8, 128), f(attn_norm[0]).reshape(4, 128),
                f(mlstm_norm[0]).reshape(4, 128), f(mlstm_skip[0]).reshape(4, 128), f(conv_b[0]).reshape(4, 128),
                f(conv_w[0]).reshape(16, 128)]
        m = dict(shared)
        m["vecs"] = np.ascontiguousarray(np.concatenate(rows, axis=0))
        m["x"] = f(x[b])
        maps.append(m)
    return maps


def kernel(**inputs):
    x = np.asarray(inputs["x"])
    B, S_LEN, _ = x.shape
    nc = build(S_LEN)
    maps = make_in_maps(S_LEN, **inputs)
    res = run_bass_kernel_spmd(nc, maps, core_ids=list(range(B)))
    return np.stack([np.asarray(r["y"], dtype=np.float32) for r in res.results], axis=0)
```

```python
from contextlib import ExitStack
import numpy as np
import concourse.bass as bass
import concourse.mybir as mybir
from concourse.bass_utils import run_bass_kernel_spmd

F32 = mybir.dt.float32
BF16 = mybir.dt.bfloat16
ALU = mybir.AluOpType
AF = mybir.ActivationFunctionType
AX = mybir.AxisListType

D = 1024
FF = 2816
NFF = 22
TT = 512
EPS = 1e-6
NR = 6
BIG = 30000.0


class Sched:
    COMPUTE = ("pe", "act", "dve", "pool")

    def __init__(self, nc, self_sync=True):
        self.nc = nc
        self.self_sync = self_sync
        self.eng = {"pe": nc.tensor, "act": nc.scalar, "dve": nc.vector, "pool": nc.gpsimd, "sp": nc.sync}
        self.prog = {k: [] for k in self.eng}
        self.cnt = {k: 0 for k in self.COMPUTE}
        self.dcnt = {}
        self.waited = {k: {} for k in self.eng}
        self.acc = {}
        self.semkeys = set(self.COMPUTE)
        self.sems = {}

    @staticmethod
    def box(ap):
        t = ap.tensor
        name = t.name
        dims = list(ap.ap)
        esz = 2 if ap.dtype == BF16 else 4
        if type(t).__name__.startswith("DRam"):
            lo = ap.offset
            hi = lo + sum((c - 1) * abs(s) for s, c in dims) + 1
            return (name, 0, 1, lo * esz, hi * esz)
        pstride = dims[0][0]
        pcnt = dims[0][1]
        if pstride == 0:
            p0, f0 = 0, ap.offset
        else:
            p0 = ap.offset // pstride
            f0 = ap.offset - p0 * pstride
        ext = sum((c - 1) * abs(s) for s, c in dims[1:]) + 1
        lo, hi = f0 * esz, (f0 + ext) * esz
        if type(t).__name__.startswith("PSum"):
            return (name, 0, 128, (lo // 2048) * 2048, -(-hi // 2048) * 2048)
        return (name, p0, p0 + pcnt, lo, hi)

    @staticmethod
    def _ov(a, b):
        return a[1] < b[2] and b[1] < a[2] and a[3] < b[4] and b[3] < a[4]

    @staticmethod
    def _contains(a, b):
        return a[1] <= b[1] and b[2] <= a[2] and a[3] <= b[3] and b[4] <= a[4]

    def _deps(self, reads, writes):
        deps = []
        for b in reads:
            for rec in self.acc.get(b[0], ()):
                if rec[0] == "W" and self._ov(rec[1], b):
                    deps.append(rec[2])
        for b in writes:
            for rec in self.acc.get(b[0], ()):
                if self._ov(rec[1], b):
                    deps.append(rec[2])
        return deps

    def _record(self, reads, writes, comp):
        for b in writes:
            lst = self.acc.setdefault(b[0], [])
            lst[:] = [r for r in lst if not self._contains(b, r[1])]
            lst.append(("W", b, comp))
        for b in reads:
            lst = self.acc.setdefault(b[0], [])
            lst[:] = [r for r in lst if not (r[0] == "R" and r[2][0] == comp[0] and r[2][1] <= comp[1]
                                             and self._contains(b, r[1]))]
            lst.append(("R", b, comp))

    def _emit_waits(self, e, deps):
        best = {}
        for k, v in deps:
            if v > best.get(k, 0):
                best[k] = v
        for k, v in best.items():
            if k == e and (e == "pe" or not self.self_sync):
                continue
            if k == e and v > self.cnt[e]:
                continue
            if self.waited[e].get(k, 0) >= v:
                continue
            self.waited[e][k] = v
            self.prog[e].append(("wait", k, v))

    def op(self, e, fn, reads=(), writes=(), inc=True):
        rb = [self.box(a) for a in reads]
        wb = [self.box(a) for a in writes]
        self._emit_waits(e, self._deps(rb, wb))
        comp = (e, self.cnt[e] + 1)
        self.prog[e].append(("op", fn, e if inc else None))
        self._record(rb, wb, comp)
        if inc:
            self.cnt[e] += 1
        return comp

    def dma(self, q, out, in_, key, **kw):
        rb = [self.box(in_)]
        wb = [self.box(out)]
        self._emit_waits(q, self._deps(rb, wb))
        self.semkeys.add(key)
        n = self.dcnt.get(key, 0) + 1
        self.dcnt[key] = n
        comp = (key, 16 * n)

        def fn(eng, out=out, in_=in_, kw=kw):
            return eng.dma_start(out=out, in_=in_, **kw)
        self.prog[q].append(("dma", fn, key))
        self._record(rb, wb, comp)
        return comp

    def final_wait(self, e="sp"):
        deps = [(k, 16 * n) for k, n in self.dcnt.items()]
        deps += [(k, self.cnt[k]) for k in self.COMPUTE if self.cnt[k] > 0 and k != e]
        self._emit_waits(e, deps)

    def emit(self, es):
        nc = self.nc
        for k in sorted(self.semkeys, key=str):
            self.sems[k] = es.enter_context(nc.semaphore("s_" + str(k)))
        block = es.enter_context(nc.Block())

        def make(e):
            def body(eng):
                for item in self.prog[e]:
                    if item[0] == "wait":
                        eng.wait_ge(self.sems[item[1]], item[2])
                    elif item[0] == "op":
                        ins = item[1](eng)
                        if item[2] is not None:
                            ins.then_inc(self.sems[item[2]], 1)
                    else:
                        item[1](eng).then_inc(self.sems[item[2]], 16)
            return body
        for e, reg in (("sp", block.sync), ("act", block.scalar), ("dve", block.vector),
                       ("pool", block.gpsimd), ("pe", block.tensor)):
            if self.prog[e]:
                reg(make(e))


class _Stop(Exception):
    pass


def build(S_LEN, dbg=False, stage=99):
    NT = S_LEN // TT
    NKT = S_LEN // 128
    NB = S_LEN // 256
    nc = bass.Bass("TRN2", target_bir_lowering=False)

    def din(name, shape):
        return nc.dram_tensor(name, shape, F32, kind="ExternalInput").ap()

    x = din("x", [S_LEN, D])
    vecs = din("vecs", [72, 128])
    bada = din("bada", [72, 128])
    w_ada = din("w_ada", [D, 9 * D])
    f1g, f1u, f1d = din("f1g", [D, FF]), din("f1u", [D, FF]), din("f1d", [FF, D])
    f2g, f2u, f2d = din("f2g", [D, FF]), din("f2u", [D, FF]), din("f2d", [FF, D])
    w_in = din("w_in", [D, 2560])
    w_out = din("w_out", [D, D])
    wqkv = din("wqkv", [3, 4, 128, 128])
    w_if = din("w_if", [1536, 8])
    b_if = din("b_if", [8])
    y = nc.dram_tensor("y", [S_LEN, D], F32, kind="ExternalOutput").ap()

    es = ExitStack()
    with es:
        S = Sched(nc)

        def sb(name, shape, dt=F32):
            return es.enter_context(nc.sbuf_tensor(name, shape, dt))

        ident_f = sb("ident_f", [128, 128])
        ones_m = sb("ones_m", [128, 128], BF16)
        ones_f = sb("ones_f", [128, 128])
        tri_f = sb("tri_f", [128, 128])
        causal4 = sb("causal4", [128, 4, 512], BF16)
        ind16 = sb("ind16", [80, 16, 128], BF16)
        E01 = sb("E01", [128, 32])
        ELB = sb("ELB", [128, 32])
        OWN = sb("OWN", [128, 32])
        vecT = sb("vecT", [128, 72])
        modT = sb("modT", [128, 72])
        prm = sb("prm", [128, 9, 8])
        modrow = sb("modrow", [1, 2, 256])
        wqkv_b = sb("wqkv_b", [128, 12, 128], BF16)
        wif_sb = sb("wif_sb", [128, 12, 8])
        wg_b = sb("wg_b", [128, 8, 8], BF16)
        bif_bc = sb("bif_bc", [128, 8])
        Kc = sb("Kc", [128, 4, S_LEN], BF16)
        Vc = sb("Vc", [128, NKT, 8, 65], BF16)
        km_f = sb("km_f", [128, 4, 16])
        km_hi = sb("km_hi", [128, 4, 16], BF16)
        km_lo = sb("km_lo", [128, 4, 16], BF16)
        Cst = sb("Cst", [128, 4, 129])
        Cb = sb("Cb", [128, 4, 129], BF16)
        xm_halo = sb("xm_halo", [128, 4, 3], BF16)
        xT = sb("xT", [128, 8, TT])
        hT = sb("hT", [128, 8, TT], BF16)
        tmpf = sb("tmpf", [128, 2, TT])
        rstd = sb("rstd", [128, 2, TT])
        ring = sb("ring", [128, NR * 2048], BF16)
        UB = 66 * 1024
        U = sb("U", [128, UB // 2], BF16)
        Uf = U.bitcast(F32)
        ringf = ring.bitcast(F32)
        stg = ringf[:, 0:1536].rearrange("p (a e) -> p a e", a=12)
        wqkvT = ringf[:, 1536:3072].rearrange("p (a e) -> p a e", a=12)
        PS = [es.enter_context(nc.psum_tensor(f"ps{i}", [128, 512], F32)) for i in range(8)]

        class Carver:
            def __init__(self):
                self.off = 0

            def b(self, n):
                o = self.off
                self.off += -(-n * 2 // 64) * 64
                assert self.off <= UB, self.off
                return U[:, o // 2:o // 2 + n]

            def f(self, n):
                o = self.off
                self.off += -(-n * 4 // 64) * 64
                assert self.off <= UB, self.off
                return Uf[:, o // 4:o // 4 + n]

        cv = Carver()
        act = cv.b(NFF * TT).rearrange("p (j t) -> p j t", j=NFF)
        x_tm = cv.f(4 * D).rearrange("p (s d) -> p s d", s=4)
        cv_mark = cv.off
        cv.off = 0
        sq = cv.b(8 * TT).rearrange("p (c t) -> p c t", c=8)
        cv.off = 0
        Qn = cv.b(4 * TT).rearrange("p (c t) -> p c t", c=4)
        xm_b = cv.b(4 * 515).rearrange("p (c t) -> p c t", c=4)
        sz = cv.b(4 * TT).rearrange("p (c t) -> p c t", c=4)
        xc_b = cv.b(4 * TT).rearrange("p (c t) -> p c t", c=4)
        xcs = cv.b(4 * TT).rearrange("p (c t) -> p c t", c=4)
        PT = cv.b(3 * TT).rearrange("p (c t) -> p c t", c=3)
        biasT = cv.b(2 * TT).rearrange("p (c t) -> p c t", c=2)
        biasq = cv.f(4 * 128).rearrange("p (s h k) -> p s h k", s=4, h=8)
        gm = cv.f(128).rearrange("p (h k) -> p h k", h=8)
        sel = cv.f(128).rearrange("p (h k) -> p h k", h=8)
        thr = cv.f(64).rearrange("p (h k) -> p h k", h=8)
        convacc = cv.f(TT)
        att_f = cv.f(4 * TT).rearrange("p (c t) -> p c t", c=4)
        o_sb = [cv.f(TT), cv.f(TT)]
        lr_f = [cv.f(TT), cv.f(TT)]
        QtS = [cv.b(512).rearrange("p (h t) -> p h t", h=4)] * 2
        KtS = [cv.b(512).rearrange("p (h t) -> p h t", h=4)] * 2
        Ktok = [cv.b(512).rearrange("p (h t) -> p h t", h=4)] * 2
        Vp = [cv.b(4 * 129).rearrange("p (h t) -> p h t", h=4)] * 2
        Sc = [cv.b(512).rearrange("p (h t) -> p h t", h=4)] * 2
        hn = [cv.f(512).rearrange("p (h t) -> p h t", h=4)] * 2
        gsm = [cv.f(64)] * 2
        bst = cv.f(4 * 6).rearrange("p (h k) -> p h k", h=4)
        mv = cv.f(8).rearrange("p (h k) -> p h k", h=4)
        ctmp = cv.f(4 * 129).rearrange("p (h t) -> p h t", h=4)
        o_cat = cv.b(8 * TT).rearrange("p (c t) -> p c t", c=8)
        wa_st = x_tm.rearrange("p s (k n) -> p (s k) n", n=256)
        wa_bufs = [wa_st[:, 0:8, :], wa_st[:, 8:16, :]]

        def mm(out, lhsT, rhs, start, stop, inc=None):
            inc = stop if inc is None else inc
            S.op("pe", lambda e: e.matmul(out, lhsT=lhsT, rhs=rhs, start=start, stop=stop),
                 reads=[lhsT, rhs], writes=[out], inc=inc)

        def tr(out, in_, ident, inc=True):
            S.op("pe", lambda e: e.transpose(out=out, in_=in_, identity=ident),
                 reads=[in_, ident], writes=[out], inc=inc)

        def actf(out, in_, func, bias=0.0, scale=1.0, extra_reads=()):
            S.op("act", lambda e: e.activation(out=out, in_=in_, func=func, bias=bias, scale=scale),
                 reads=[in_] + list(extra_reads), writes=[out])

        def acopy(out, in_):
            S.op("act", lambda e: e.copy(out=out, in_=in_), reads=[in_], writes=[out])

        def vcopy(out, in_):
            S.op("dve", lambda e: e.tensor_copy(out=out, in_=in_), reads=[in_], writes=[out])

        def vtt(out, in0, in1, op):
            S.op("dve", lambda e: e.tensor_tensor(out=out, in0=in0, in1=in1, op=op), reads=[in0, in1], writes=[out])

        def vts(out, in0, s1, s2, op0, op1=None, rd=()):
            if op1 is None:
                S.op("dve", lambda e: e.tensor_scalar(out=out, in0=in0, scalar1=s1, scalar2=None, op0=op0),
                     reads=[in0] + list(rd), writes=[out])
            else:
                S.op("dve", lambda e: e.tensor_scalar(out=out, in0=in0, scalar1=s1, scalar2=s2, op0=op0, op1=op1),
                     reads=[in0] + list(rd), writes=[out])

        def vstt(out, in0, scalar, in1, op0, op1, rd=()):
            S.op("dve", lambda e: e.scalar_tensor_tensor(out=out, in0=in0, scalar=scalar, in1=in1, op0=op0, op1=op1),
                 reads=[in0, in1] + list(rd), writes=[out])

        def pmemset(ap, v):
            S.op("pool", lambda e: e.memset(ap, v), writes=[ap])

        def vmemset(ap, v):
            S.op("dve", lambda e: e.memset(ap, v), writes=[ap])

        def pasel(ap, pattern, cmp, cm, base=0):
            S.op("pool", lambda e: e.affine_select(out=ap, in_=ap, pattern=pattern, compare_op=cmp, fill=0.0,
                                                   base=base, channel_multiplier=cm), reads=[ap], writes=[ap])

        dumps = {}

        def chk(st):
            if stage <= st:
                raise _Stop()

        def dump(name, ap, dt=F32):
            if not dbg or name in dumps:
                return
            shp = list(ap.shape)
            dumps[name] = nc.dram_tensor("dbg_" + name, shp, dt, kind="ExternalOutput").ap()
            S.dma("sp", dumps[name], ap, "dbg_" + name)

        pmemset(ident_f[:], 1.0)
        pasel(ident_f[:], [[-1, 128]], ALU.is_equal, 1)
        pmemset(ones_m[:], 1.0 / D)
        pmemset(ones_f[:], 1.0)
        pmemset(tri_f[:], 1.0)
        pasel(tri_f[:], [[1, 128]], ALU.is_ge, -1)
        pmemset(causal4[:], 1.0)
        pasel(causal4[:], [[-128, 4], [1, 512]], ALU.is_ge, -1)
        pmemset(ind16[:], 1.0)
        pasel(ind16[0:16], [[1, 16], [0, 128]], ALU.is_equal, -1)
        pasel(ind16[64:80], [[1, 16], [0, 128]], ALU.is_equal, -1)
        pmemset(E01[:, 0:16], 1.0)
        pmemset(E01[:, 16:32], 0.0)
        pmemset(ELB[:, 0:16], 0.0)
        pmemset(ELB[:, 16:32], -1e30)
        pmemset(OWN[:], 0.0)
        pmemset(OWN[:, 16:17], 1.0)
        pmemset(Vc[:, :, :, 64:65], 1.0)
        pmemset(km_f[:], 0.0)
        pmemset(km_hi[:], 0.0)
        pmemset(km_lo[:], 0.0)
        pmemset(Cst[:], 0.0)
        pmemset(Cb[:], 0.0)
        pmemset(xm_halo[:], 0.0)

        S.dma("sp", stg[0:72, 0, :], vecs, "ldv1")
        tr(PS[0][:, 0:72], stg[0:72, 0, :], ident_f[0:72, 0:72])
        vcopy(vecT[:], PS[0][:, 0:72])
        S.dma("sp", stg[0:72, 1, :], bada, "ldv2")
        tr(PS[1][:, 0:72], stg[0:72, 1, :], ident_f[0:72, 0:72])
        vcopy(modT[:], PS[1][:, 0:72])
        S.dma("sp", bif_bc[:], b_if.partition_broadcast(128), "ldv3")
        S.dma("sp", wif_sb[:], w_if.rearrange("(j p) g -> p j g", p=128), "ldv4")

        NG = 36
        for g in range(NG):
            buf = wa_bufs[g % 2]
            S.dma("sp", buf, w_ada[:, g * 256:(g + 1) * 256].rearrange("(kc p) n -> p kc n", p=128), f"lda{g % 2}")
            pso = PS[2 + g % 2]
            for kc in range(8):
                mm(pso[0:1, 0:256], vecT[:, kc:kc + 1], buf[:, kc, :], kc == 0, kc == 7)
            acopy(modrow[0:1, g % 2, :], pso[0:1, 0:256])
            for jj in range(2):
                j = 2 * g + jj
                mm(PS[4][:, j:j + 1], modrow[0:1, g % 2, jj * 128:(jj + 1) * 128], ones_f[0:1, 0:1], True, True)
        vtt(modT[:], modT[:], PS[4][:, 0:72], ALU.add)

        def modv(i):
            return modT[:, i * 8:(i + 1) * 8]

        def vrow(r, n=8):
            return vecT[:, r:r + n]
        for blk, (nrow, half) in enumerate(((8, 0.5), (16, 1.0), (24, 0.5))):
            vstt(prm[:, 3 * blk + 0, :], modv(3 * blk + 1), 1.0, vrow(nrow), ALU.add, ALU.mult)
            vcopy(prm[:, 3 * blk + 1, :], modv(3 * blk + 0))
            vts(prm[:, 3 * blk + 2, :], modv(3 * blk + 2), 1.0, half, ALU.add, ALU.mult)
        fin_g = vrow(32)
        attn_g = vrow(40, 4)
        mn_g = vrow(44, 4)
        skip_g = vrow(48, 4)
        conv_bv = vrow(52, 4)

        def conv_wv(j, hc):
            return vecT[:, 56 + j * 4 + hc:57 + j * 4 + hc]

        S.dma("sp", stg[:], wqkv.rearrange("a h d e -> d (a h) e"), "ldv5")
        vcopy(wqkv_b[:], stg[:])
        for i in range(12):
            tr(PS[5 + i % 2][:, 0:128], stg[:, i, :], ident_f[:])
            acopy(wqkvT[:, i, :], PS[5 + i % 2][:, 0:128])
        for h in range(4):
            mm(PS[7][:, h * 8:(h + 1) * 8], wqkvT[:, h, :], wif_sb[:, 3 * h, :], True, False)
            mm(PS[7][:, h * 8:(h + 1) * 8], wqkvT[:, 4 + h, :], wif_sb[:, 3 * h + 1, :], False, True, inc=False)
            mm(PS[7][:, (4 + h) * 8:(5 + h) * 8], wqkvT[:, 8 + h, :], wif_sb[:, 3 * h + 2, :], True, True, inc=(h == 3))
        vcopy(wg_b[:].rearrange("p a g -> p (a g)"), PS[7][:, 0:64])

        units = []

        def add_unit_cols(w, c0):
            units.append(w[:, c0:c0 + 256].rearrange("(kc p) n -> p kc n", p=128))

        def add_unit_rows(w, r0):
            units.append(w[r0:r0 + 256, :].rearrange("(jj p) n -> p jj n", p=128))

        for t in range(NT):
            for g in range(11):
                add_unit_cols(f1g, g * 256)
                add_unit_cols(f1u, g * 256)
            for g in range(11):
                add_unit_rows(f1d, g * 256)
            for g in range(10):
                add_unit_cols(w_in, g * 256)
            for g in range(4):
                add_unit_cols(w_out, g * 256)
            for g in range(11):
                add_unit_cols(f2g, g * 256)
                add_unit_cols(f2u, g * 256)
            for g in range(11):
                add_unit_rows(f2d, g * 256)
        wstate = {"issued": 0, "next": 0}

        def wview(u, rows):
            slot = ring[:, (u % NR) * 2048:(u % NR + 1) * 2048]
            if rows:
                return slot.rearrange("p (jj n) -> p jj n", jj=2)
            return slot.rearrange("p (kc n) -> p kc n", kc=8)

        def wissue_upto(n):
            while wstate["issued"] < min(n, len(units)):
                u = wstate["issued"]
                src = units[u]
                rows = (src.shape[1] == 2)
                S.dma("pool", wview(u, rows), src, f"w{u % NR}")
                wstate["issued"] += 1

        def wnext(rows=False):
            u = wstate["next"]
            wstate["next"] += 1
            wissue_upto(u + NR - 1)
            return wview(u, rows)

        def rms_mod(ns, sh):
            for c in range(8):
                actf(sq[:, c, :], xT[:, c, :], AF.Square)
            for c in range(8):
                mm(PS[6][:], ones_m[:], sq[:, c, :], c == 0, c == 7)
            actf(rstd[:, 0, :], PS[6][:], AF.Sqrt, bias=eps_t[:, 0:1], scale=1.0, extra_reads=[eps_t[:, 0:1]])
            S.op("dve", lambda e: e.reciprocal(out=rstd[:, 1, :], in_=rstd[:, 0, :]),
                 reads=[rstd[:, 0, :]], writes=[rstd[:, 1, :]])
            for c in range(8):
                vstt(tmpf[:, c % 2, :], xT[:, c, :], ns[:, c:c + 1], rstd[:, 1, :], ALU.mult, ALU.mult,
                     rd=[ns[:, c:c + 1]])
                actf(hT[:, c, :], tmpf[:, c % 2, :], AF.Identity, bias=sh[:, c:c + 1], scale=1.0,
                     extra_reads=[sh[:, c:c + 1]])

        def ffn(rg):
            for g in range(11):
                wg_v = wnext()
                wu_v = wnext()
                for jj in range(2):
                    j = 2 * g + jj
                    pg, pu = PS[j % 2], PS[2 + j % 2]
                    for kc in range(8):
                        mm(pg[:], wg_v[:, kc, jj * 128:(jj + 1) * 128], hT[:, kc, :], kc == 0, kc == 7)
                    for kc in range(8):
                        mm(pu[:], wu_v[:, kc, jj * 128:(jj + 1) * 128], hT[:, kc, :], kc == 0, kc == 7)
                    actf(tmpf[:, j % 2, :], pg[:], AF.Silu)
                    vtt(act[:, j, :], tmpf[:, j % 2, :], pu[:], ALU.mult)
            dump('act', act[:], BF16)
            for g in range(11):
                wd_v = wnext(rows=True)
                for jj in range(2):
                    j = 2 * g + jj
                    for i in range(8):
                        mm(PS[i][:], wd_v[:, jj, i * 128:(i + 1) * 128], act[:, j, :], j == 0, j == NFF - 1,
                           inc=(i == 7 and jj == 1))
            for i in range(8):
                vstt(xT[:, i, :], PS[i][:], rg[:, i:i + 1], xT[:, i, :], ALU.mult, ALU.add, rd=[rg[:, i:i + 1]])

        eps_t = sb("eps_t", [128, 1])
        pmemset(eps_t[:], EPS)

        def mixer(t):
            c0 = t * TT
            for g in range(4):
                wv_ = wnext()
                for jj in range(2):
                    oc = 2 * g + jj
                    ps = PS[4 + oc % 2]
                    for kc in range(8):
                        mm(ps[:], wv_[:, kc, jj * 128:(jj + 1) * 128], hT[:, kc, :], kc == 0, kc == 7)
                    if oc < 4:
                        acopy(Qn[:, oc, :], ps[:])
                    else:
                        vcopy(Kc[:, oc - 4, c0:c0 + TT], ps[:])
            for g in range(2):
                wv_ = wnext()
                for s4 in range(4):
                    ps = PS[6 + s4 % 2]
                    for kc in range(8):
                        mm(ps[:, 0:256], hT[:, kc, s4 * 128:(s4 + 1) * 128], wv_[:, kc, :], kc == 0, kc == 7)
                    dst = Vc[:, t * 4 + s4, g * 4:(g + 1) * 4, 0:64]
                    src = ps[:, 0:256].rearrange("p (h e) -> p h e", h=4)
                    if s4 % 2 == 0:
                        acopy(dst, src)
                    else:
                        vcopy(dst, src)
            vcopy(xm_b[:, :, 0:3], xm_halo[:])
            for g in range(2):
                wv_ = wnext()
                for jj in range(2):
                    oc = 2 * g + jj
                    ps = PS[4 + oc % 2]
                    for kc in range(8):
                        mm(ps[:], wv_[:, kc, jj * 128:(jj + 1) * 128], hT[:, kc, :], kc == 0, kc == 7)
                    acopy(xm_b[:, oc, 3:515], ps[:])
            vcopy(xm_halo[:], xm_b[:, :, 512:515])
            for g in range(2):
                wv_ = wnext()
                for jj in range(2):
                    oc = 2 * g + jj
                    ps = PS[6 + oc % 2]
                    for kc in range(8):
                        mm(ps[:], wv_[:, kc, jj * 128:(jj + 1) * 128], hT[:, kc, :], kc == 0, kc == 7)
                    actf(sz[:, oc, :], ps[:], AF.Silu)
            for hc in range(4):
                vts(convacc, xm_b[:, hc, 0:512], conv_wv(0, hc), conv_bv[:, hc:hc + 1], ALU.mult, ALU.add,
                    rd=[conv_wv(0, hc), conv_bv[:, hc:hc + 1]])
                for j in range(1, 4):
                    vstt(convacc, xm_b[:, hc, j:j + 512], conv_wv(j, hc), convacc, ALU.mult, ALU.add,
                         rd=[conv_wv(j, hc)])
                actf(xc_b[:, hc, :], convacc, AF.Silu)
                vts(xcs[:, hc, :], xc_b[:, hc, :], skip_g[:, hc:hc + 1], None, ALU.mult, rd=[skip_g[:, hc:hc + 1]])
            dump('Qn', Qn[:], BF16); dump('xm_b', xm_b[:], BF16); dump('sz', sz[:], BF16); dump('xc_b', xc_b[:], BF16)
            chk(3)
            attention(t)
            chk(4)
            dump('att_f', att_f[:]); dump('biasq', biasq[:])
            mlstm(t)
            chk(5)
            dump('o_cat', o_cat[:], BF16)
            rg = prm[:, 5, :]
            for g in range(4):
                wv_ = wnext()
                for jj in range(2):
                    oc = 2 * g + jj
                    ps = PS[4 + oc % 2]
                    for kc in range(8):
                        mm(ps[:], wv_[:, kc, jj * 128:(jj + 1) * 128], o_cat[:, kc, :], kc == 0, kc == 7)
                    vstt(xT[:, oc, :], ps[:], rg[:, oc:oc + 1], xT[:, oc, :], ALU.mult, ALU.add, rd=[rg[:, oc:oc + 1]])

        def attention(t):
            c0 = t * TT
            for c in range(4):
                for b in range(2):
                    blk = 2 * t + b
                    src = Kc[:, c, blk * 256:(blk + 1) * 256]
                    S.op("dve", lambda e, src=src, c=c, blk=blk: e.tensor_reduce(
                        out=km_f[:, c, blk:blk + 1], in_=src, axis=AX.X, op=ALU.add),
                        reads=[src], writes=[km_f[:, c, blk:blk + 1]])
                kk = km_f[:, c, 2 * t:2 * t + 2]
                vts(kk, kk, 1.0 / 256, None, ALU.mult)
                vcopy(km_hi[:, c, 2 * t:2 * t + 2], kk)
                vtt(kk, kk, km_hi[:, c, 2 * t:2 * t + 2], ALU.subtract)
                vcopy(km_lo[:, c, 2 * t:2 * t + 2], kk)
            chk(3.1)
            for s4 in range(4):
                qb = 2 * t + s4 // 2
                for h in range(8):
                    c, r0 = h // 2, 64 * (h % 2)
                    psG = PS[6 + h % 2]
                    qsl = Qn[r0:r0 + 64, c, s4 * 128:(s4 + 1) * 128]
                    mm(psG[:, c * 16:(c + 1) * 16], qsl, km_hi[r0:r0 + 64, c, :], True, False)
                    mm(psG[:, c * 16:(c + 1) * 16], qsl, km_lo[r0:r0 + 64, c, :], False, True, inc=(h >= 6))
                chk(3.11)
                elb = ELB[:, 16 - qb:32 - qb].unsqueeze(1).broadcast_to([128, 8, 16])
                e01 = E01[:, 16 - qb:32 - qb].unsqueeze(1).broadcast_to([128, 8, 16])
                own = OWN[:, 16 - qb:32 - qb].unsqueeze(1).broadcast_to([128, 8, 16])
                gm4 = gm.rearrange("p (c two) k -> p c two k", two=2)
                for par in range(2):
                    vtt(gm4[:, :, par, :], PS[6 + par][:, 0:64].rearrange("p (c k) -> p c k", c=4),
                        ELB[:, 16 - qb:32 - qb].unsqueeze(1).broadcast_to([128, 4, 16]), ALU.add)
                chk(3.12)
                for h in range(8):
                    S.op("dve", lambda e, h=h: e.max(out=thr[:, h, :], in_=gm[:, h, :]),
                         reads=[gm[:, h, :]], writes=[thr[:, h, :]])
                chk(3.13)
                vtt(sel, gm, thr[:, :, 2:3].broadcast_to([128, 8, 16]), ALU.is_ge)
                chk(3.14)
                vtt(sel, sel, e01, ALU.mult)
                vtt(sel, sel, own, ALU.add)
                vts(biasq[:, s4, :, :], sel, 1.0, BIG, ALU.subtract, ALU.mult)
                chk(3.15)
                if s4 == 1:
                    chk(3.16)
            chk(3.2)
            nkt = 4 * (t + 1)
            LA = 2
            pend = None
            for h in range(8):
                c, r0 = h // 2, 64 * (h % 2)
                bT = biasT[:, h % 2, :]
                psT = PS[7]
                for s4 in range(4):
                    tr(psT[0:16, s4 * 128:(s4 + 1) * 128], biasq[:, s4, h, :], ident_f[:], inc=(s4 == 3))
                vcopy(bT[r0:r0 + 16, :], psT[0:16, :])
                pso = PS[3 + h % 2]

                def qk(kt, c=c, r0=r0, bT=bT):
                    pss = PS[kt % 3]
                    mm(pss[:], Kc[r0:r0 + 64, c, kt * 128:(kt + 1) * 128], Qn[r0:r0 + 64, c, :], True, False)
                    mm(pss[:], ind16[r0:r0 + 16, kt // 2, :], bT[r0:r0 + 16, :], False, True)

                for kt in range(min(LA, nkt)):
                    qk(kt)
                if pend is not None:
                    pend()
                    pend = None
                for kt in range(nkt):
                    if kt + LA < nkt:
                        qk(kt + LA)
                    pss = PS[kt % 3]
                    pt = PT[:, kt % 3, :]
                    actf(pt, pss[:], AF.Exp, scale=0.125)
                    if kt >= 4 * t:
                        vtt(pt, pt, causal4[:, kt - 4 * t, :], ALU.mult)
                    mm(pso[0:65, :], Vc[:, kt, h, :], pt, kt == 0, kt == nkt - 1, inc=True)
                lr, osb, psb = lr_f[h % 2], o_sb[h % 2], PS[5 + h % 2]
                actf(lr[32:33, :], pso[64:65, :], AF.Ln)
                actf(lr[64:65, :], lr[32:33, :], AF.Exp, scale=-1.0)
                acopy(osb[0:64, :], pso[0:64, :])

                def ep(lr=lr, osb=osb, psb=psb, c=c, r0=r0):
                    mm(psb[0:64, :], ones_f[64:65, 0:64], lr[64:65, :], True, True)
                    vtt(att_f[r0:r0 + 64, c, :], osb[0:64, :], psb[0:64, :], ALU.mult)
                pend = ep
            pend()
            chk(3.6)
            for c in range(4):
                actf(sq[:, c, :], att_f[:, c, :], AF.Square, scale=2.0 ** 0.5)
            for c in range(4):
                mm(PS[6][:], ones_m[:], sq[:, c, :], c == 0, c == 3)
            actf(rstd[:, 0, :], PS[6][:], AF.Sqrt, bias=eps_t[:, 0:1], scale=1.0, extra_reads=[eps_t[:, 0:1]])
            S.op("dve", lambda e: e.reciprocal(out=rstd[:, 1, :], in_=rstd[:, 0, :]),
                 reads=[rstd[:, 0, :]], writes=[rstd[:, 1, :]])
            for c in range(4):
                vstt(o_cat[:, c, :], att_f[:, c, :], attn_g[:, c:c + 1], rstd[:, 1, :], ALU.mult, ALU.mult,
                     rd=[attn_g[:, c:c + 1]])

        def mlstm(t):
            for ch in range(4):
                cs = slice(ch * 128, (ch + 1) * 128)
                cs3 = slice(3 + ch * 128, 3 + (ch + 1) * 128)
                b2 = ch % 2
                gs = gsm[b2]
                psg = PS[0]
                for h in range(4):
                    mm(psg[:, 0:8], xc_b[:, h, cs], wg_b[:, h, :], h == 0, False)
                for h in range(4):
                    mm(psg[:, 0:8], xm_b[:, h, cs3], wg_b[:, 4 + h, :], False, h == 3)
                vtt(gs[:, 0:8], psg[:, 0:8], bif_bc[:], ALU.add)
                actf(gs[:, 28:32], gs[:, 4:8], AF.Exp, scale=-1.0)
                actf(gs[:, 4:8], gs[:, 28:32], AF.Ln, bias=1.0)
                vts(gs[:, 4:8], gs[:, 4:8], -1.0, None, ALU.mult)
                mm(PS[0][:, 16:20], tri_f[:], gs[:, 4:8], True, True)
                mm(PS[0][:, 24:28], ones_f[:], gs[:, 4:8], True, True)
                vcopy(gs[:, 8:16].rearrange("p (a k) -> p a k", a=2),
                      PS[0][:, 16:32].rearrange("p (a k) -> p a k", a=2)[:, :, 0:4])
                vtt(gs[:, 28:32], gs[:, 0:4], gs[:, 8:12], ALU.subtract)
                actf(gs[:, 16:20], gs[:, 28:32], AF.Exp)
                actf(gs[:, 20:28], gs[:, 8:16], AF.Exp)
                pq, pk, pkt, pvt = PS[1], PS[2], PS[3], PS[4]
                for h in range(4):
                    mm(pq[:, h * 128:(h + 1) * 128], wqkv_b[:, h, :], xc_b[:, h, cs], True, True, inc=(h == 3))
                for h in range(4):
                    mm(pk[:, h * 128:(h + 1) * 128], wqkv_b[:, 4 + h, :], xc_b[:, h, cs], True, True, inc=(h == 3))
                for h in range(4):
                    mm(pkt[:, h * 128:(h + 1) * 128], xc_b[:, h, cs], wqkv_b[:, 4 + h, :], True, True, inc=(h == 3))
                for h in range(4):
                    mm(pvt[:, h * 128:(h + 1) * 128], xm_b[:, h, cs3], wqkv_b[:, 8 + h, :], True, True, inc=(h == 3))
                kscale = 128.0 ** -0.5
                acopy(QtS[b2].rearrange("p h t -> p (h t)"), pq[:])
                actf(KtS[b2].rearrange("p h t -> p (h t)"), pk[:], AF.Copy, scale=kscale)
                actf(Ktok[b2].rearrange("p h t -> p (h t)"), pkt[:], AF.Copy, scale=kscale)
                a_bc = gs[:, 16:20].unsqueeze(2).broadcast_to([128, 4, 128])
                vtt(Vp[b2][:, :, 0:128], pvt[:].rearrange("p (h e) -> p h e", h=4), a_bc, ALU.mult)
                vcopy(Vp[b2][:, :, 128:129], gs[:, 16:20].unsqueeze(2))
                pS = PS[5]
                for h in range(4):
                    mm(pS[:, h * 128:(h + 1) * 128], KtS[b2][:, h, :], QtS[b2][:, h, :], True, True, inc=(h == 3))
                vtt(Sc[b2][:], pS[:].rearrange("p (h t) -> p h t", h=4),
                    tri_f[:].unsqueeze(1).broadcast_to([128, 4, 128]), ALU.mult)
                pH = [PS[6], PS[7]]
                for h in range(4):
                    o = pH[h // 2][:, (h % 2) * 129:(h % 2) * 129 + 129]
                    mm(o, Sc[b2][:, h, :], Vp[b2][:, h, :], True, False)
                    mm(o, QtS[b2][:, h, :], Cb[:, h, :], False, True, inc=(h % 2 == 1))
                pU = [PS[1], PS[2]]
                for h in range(4):
                    o = pU[h // 2][:, (h % 2) * 129:(h % 2) * 129 + 129]
                    mm(o, Ktok[b2][:, h, :], Vp[b2][:, h, :], True, True, inc=(h % 2 == 1))
                for h in range(4):
                    hp, hh = h // 2, h % 2
                    S.op("dve", lambda e, hp=hp, hh=hh, h=h: e.bn_stats(out=bst[:, h, :], in_=pH[hp][:, hh * 129:hh * 129 + 128]),
                         reads=[pH[hp][:, hh * 129:hh * 129 + 128]], writes=[bst[:, h, :]])
                for hp in range(2):
                    Hv = pH[hp][:, 0:258].rearrange("p (h v) -> p h v", h=2)
                    vtt(gs[:, 28 + 2 * hp:30 + 2 * hp].unsqueeze(2), Hv[:, :, 128:129],
                        gs[:, 20 + 2 * hp:22 + 2 * hp].unsqueeze(2), ALU.mult)
                for h in range(4):
                    S.op("dve", lambda e, h=h: e.bn_aggr(out=mv[:, h, :], in_=bst[:, h, :]),
                         reads=[bst[:, h, :]], writes=[mv[:, h, :]])
                vts(gs[:, 52:56], gs[:, 28:32], -1.0, None, ALU.mult)
                vtt(gs[:, 32:36], gs[:, 28:32], gs[:, 52:56], ALU.max)
                vts(gs[:, 32:36], gs[:, 32:36], 1.0, None, ALU.max)
                S.op("dve", lambda e, gs=gs: e.reciprocal(out=gs[:, 36:40], in_=gs[:, 32:36]),
                     reads=[gs[:, 32:36]], writes=[gs[:, 36:40]])
                vtt(gs[:, 36:40], gs[:, 36:40], gs[:, 20:24], ALU.mult)
                vtt(gs[:, 40:44], gs[:, 36:40], gs[:, 36:40], ALU.mult)
                vtt(gs[:, 40:44].unsqueeze(2), gs[:, 40:44].unsqueeze(2), mv[:, :, 1:2], ALU.mult)
                actf(gs[:, 44:48], gs[:, 40:44], AF.Sqrt, bias=eps_t[:, 0:1], scale=1.0, extra_reads=[eps_t[:, 0:1]])
                S.op("dve", lambda e, gs=gs: e.reciprocal(out=gs[:, 48:52], in_=gs[:, 44:48]),
                     reads=[gs[:, 44:48]], writes=[gs[:, 48:52]])
                vtt(gs[:, 48:52], gs[:, 48:52], gs[:, 36:40], ALU.mult)
                for h in range(4):
                    hp, hh = h // 2, h % 2
                    vts(hn[b2][:, h, :], pH[hp][:, hh * 129:hh * 129 + 128], mv[:, h, 0:1], gs[:, 48 + h:49 + h],
                        ALU.subtract, ALU.mult, rd=[mv[:, h, 0:1], gs[:, 48 + h:49 + h]])
                for hp in range(2):
                    vtt(ctmp[:, 2 * hp:2 * hp + 2, :], Cst[:, 2 * hp:2 * hp + 2, :],
                        pU[hp][:, 0:258].rearrange("p (h v) -> p h v", h=2), ALU.add)
                vtt(Cst[:], ctmp[:], gs[:, 24:28].unsqueeze(2).broadcast_to([128, 4, 129]), ALU.mult)
                vcopy(Cb[:], Cst[:])
                pT = PS[3]
                for h in range(4):
                    tr(pT[:, h * 128:(h + 1) * 128], hn[b2][:, h, :], ident_f[:], inc=(h == 3))
                for h in range(4):
                    vstt(tmpf[:, 0, h * 128:(h + 1) * 128], pT[:, h * 128:(h + 1) * 128], mn_g[:, h:h + 1],
                         xcs[:, h, cs], ALU.mult, ALU.add, rd=[mn_g[:, h:h + 1]])
                    vtt(o_cat[:, 4 + h, cs], tmpf[:, 0, h * 128:(h + 1) * 128], sz[:, h, cs], ALU.mult)

        try:
          chk(0)
          for t in range(NT):
              c0 = t * TT
              S.dma("sp", x_tm, x[c0:c0 + TT, :].rearrange("(s p) d -> p s d", p=128), "ldx")
              for c in range(8):
                  ps = PS[6 + c % 2]
                  for s4 in range(4):
                      tr(ps[:, s4 * 128:(s4 + 1) * 128], x_tm[:, s4, c * 128:(c + 1) * 128], ident_f[:], inc=(s4 == 3))
                  acopy(xT[:, c, :], ps[:])
              dump('modT', modT[:]); dump('prm', prm[:]); dump('xT0', xT[:])
              rms_mod(prm[:, 0, :], prm[:, 1, :])
              dump('hT1', hT[:], BF16)
              chk(1)
              ffn(prm[:, 2, :])
              chk(2)
              dump('xT1', xT[:])
              rms_mod(prm[:, 3, :], prm[:, 4, :])
              dump('hT2', hT[:], BF16)
              mixer(t)
              dump('xT2', xT[:])
              rms_mod(prm[:, 6, :], prm[:, 7, :])
              ffn(prm[:, 8, :])
              for c in range(8):
                  actf(sq[:, c, :], xT[:, c, :], AF.Square)
              for c in range(8):
                  mm(PS[6][:], ones_m[:], sq[:, c, :], c == 0, c == 7)
              actf(rstd[:, 0, :], PS[6][:], AF.Sqrt, bias=eps_t[:, 0:1], scale=1.0, extra_reads=[eps_t[:, 0:1]])
              S.op("dve", lambda e: e.reciprocal(out=rstd[:, 1, :], in_=rstd[:, 0, :]),
                   reads=[rstd[:, 0, :]], writes=[rstd[:, 1, :]])
              for c in range(8):
                  vstt(tmpf[:, c % 2, :], xT[:, c, :], fin_g[:, c:c + 1], rstd[:, 1, :], ALU.mult, ALU.mult,
                       rd=[fin_g[:, c:c + 1]])
                  ps = PS[4 + c % 2]
                  for s4 in range(4):
                      tr(ps[:, s4 * 128:(s4 + 1) * 128], tmpf[:, c % 2, s4 * 128:(s4 + 1) * 128], ident_f[:], inc=(s4 == 3))
                  acopy(x_tm[:, :, c * 128:(c + 1) * 128], ps[:].rearrange("p (s d) -> p s d", s=4))
              S.dma("sp", y[c0:c0 + TT, :].rearrange("(s p) d -> p s d", p=128), x_tm, "sty")
        except _Stop:
            pass
        S.final_wait("sp")
        S.emit(es)
    return nc


def make_in_maps(S_LEN, x, c, w_ada, b_ada, ffn1_norm, ffn1_w_gate, ffn1_w_up, ffn1_w_down,
                 mix_norm, w_in, conv_w, conv_b, w_q_m, w_k_m, w_v_m, w_if, b_if,
                 mlstm_norm, mlstm_skip, attn_norm, w_out,
                 ffn2_norm, ffn2_w_gate, ffn2_w_up, ffn2_w_down, final_norm):
    f = lambda a: np.ascontiguousarray(np.asarray(a, dtype=np.float32))
    B = x.shape[0]
    shared = {
        "bada": f(b_ada[0]).reshape(72, 128),
        "w_ada": f(w_ada[0]),
        "f1g": f(ffn1_w_gate[0]), "f1u": f(ffn1_w_up[0]), "f1d": f(ffn1_w_down[0]),
        "f2g": f(ffn2_w_gate[0]), "f2u": f(ffn2_w_up[0]), "f2d": f(ffn2_w_down[0]),
        "w_in": f(w_in[0]), "w_out": f(w_out[0]),
        "wqkv": f(np.stack([w_q_m[0], w_k_m[0], w_v_m[0]])),
        "w_if": f(w_if[0]), "b_if": f(b_if[0]),
    }
    maps = []
    for b in range(B):
        rows = [f(c[b]).reshape(8, 128), f(ffn1_norm[0]).reshape(8, 128), f(mix_norm[0]).reshape(8, 128),
                f(ffn2_norm[0]).reshape(8, 128), f(final_norm).reshape(8, 128), f(attn_norm[0]).reshape(4, 128),
                f(mlstm_norm[0]).reshape(4, 128), f(mlstm_skip[0]).reshape(4, 128), f(conv_b[0]).reshape(4, 128),
                f(conv_w[0]).reshape(16, 128)]
        m = dict(shared)
        m["vecs"] = np.ascontiguousarray(np.concatenate(rows, axis=0))
        m["x"] = f(x[b])
        maps.append(m)
    return maps


def kernel(**inputs):
    x = np.asarray(inputs["x"])
    B, S_LEN, _ = x.shape
    nc = build(S_LEN)
    maps = make_in_maps(S_LEN, **inputs)
    res = run_bass_kernel_spmd(nc, maps, core_ids=list(range(B)))
    return np.stack([np.asarray(r["y"], dtype=np.float32) for r in res.results], axis=0)
```

```python
from contextlib import ExitStack
import numpy as np
import concourse.bass as bass
import concourse.mybir as mybir
from concourse.bass_utils import run_bass_kernel_spmd

F32 = mybir.dt.float32
BF16 = mybir.dt.bfloat16
ALU = mybir.AluOpType
AF = mybir.ActivationFunctionType
AX = mybir.AxisListType

D = 1024
FF = 2816
NFF = 22
TT = 512
EPS = 1e-6
NR = 6
BIG = 30000.0


class Sched:
    COMPUTE = ("pe", "act", "dve", "pool")

    def __init__(self, nc, self_sync=True):
        self.nc = nc
        self.self_sync = self_sync
        self.eng = {"pe": nc.tensor, "act": nc.scalar, "dve": nc.vector, "pool": nc.gpsimd, "sp": nc.sync}
        self.prog = {k: [] for k in self.eng}
        self.cnt = {k: 0 for k in self.COMPUTE}
        self.dcnt = {}
        self.waited = {k: {} for k in self.eng}
        self.acc = {}
        self.semkeys = set(self.COMPUTE)
        self.sems = {}

    @staticmethod
    def box(ap):
        t = ap.tensor
        name = t.name
        dims = list(ap.ap)
        esz = 2 if ap.dtype == BF16 else 4
        if type(t).__name__.startswith("DRam"):
            lo = ap.offset
            hi = lo + sum((c - 1) * abs(s) for s, c in dims) + 1
            return (name, 0, 1, lo * esz, hi * esz)
        pstride = dims[0][0]
        pcnt = dims[0][1]
        if pstride == 0:
            p0, f0 = 0, ap.offset
        else:
            p0 = ap.offset // pstride
            f0 = ap.offset - p0 * pstride
        ext = sum((c - 1) * abs(s) for s, c in dims[1:]) + 1
        lo, hi = f0 * esz, (f0 + ext) * esz
        if type(t).__name__.startswith("PSum"):
            return (name, 0, 128, (lo // 2048) * 2048, -(-hi // 2048) * 2048)
        return (name, p0, p0 + pcnt, lo, hi)

    @staticmethod
    def _ov(a, b):
        return a[1] < b[2] and b[1] < a[2] and a[3] < b[4] and b[3] < a[4]

    @staticmethod
    def _contains(a, b):
        return a[1] <= b[1] and b[2] <= a[2] and a[3] <= b[3] and b[4] <= a[4]

    def _deps(self, reads, writes):
        deps = []
        for b in reads:
            for rec in self.acc.get(b[0], ()):
                if rec[0] == "W" and self._ov(rec[1], b):
                    deps.append(rec[2])
        for b in writes:
            for rec in self.acc.get(b[0], ()):
                if self._ov(rec[1], b):
                    deps.append(rec[2])
        return deps

    def _record(self, reads, writes, comp):
        for b in writes:
            lst = self.acc.setdefault(b[0], [])
            lst[:] = [r for r in lst if not self._contains(b, r[1])]
            lst.append(("W", b, comp))
        for b in reads:
            lst = self.acc.setdefault(b[0], [])
            lst[:] = [r for r in lst if not (r[0] == "R" and r[2][0] == comp[0] and r[2][1] <= comp[1]
                                             and self._contains(b, r[1]))]
            lst.append(("R", b, comp))

    def _emit_waits(self, e, deps):
        best = {}
        for k, v in deps:
            if v > best.get(k, 0):
                best[k] = v
        for k, v in best.items():
            if k == e and (e == "pe" or not self.self_sync):
                continue
            if k == e and v > self.cnt[e]:
                continue
            if self.waited[e].get(k, 0) >= v:
                continue
            self.waited[e][k] = v
            self.prog[e].append(("wait", k, v))

    def op(self, e, fn, reads=(), writes=(), inc=True):
        rb = [self.box(a) for a in reads]
        wb = [self.box(a) for a in writes]
        self._emit_waits(e, self._deps(rb, wb))
        comp = (e, self.cnt[e] + 1)
        self.prog[e].append(("op", fn, e if inc else None))
        self._record(rb, wb, comp)
        if inc:
            self.cnt[e] += 1
        return comp

    def dma(self, q, out, in_, key, **kw):
        rb = [self.box(in_)]
        wb = [self.box(out)]
        self._emit_waits(q, self._deps(rb, wb))
        self.semkeys.add(key)
        n = self.dcnt.get(key, 0) + 1
        self.dcnt[key] = n
        comp = (key, 16 * n)

        def fn(eng, out=out, in_=in_, kw=kw):
            return eng.dma_start(out=out, in_=in_, **kw)
        self.prog[q].append(("dma", fn, key))
        self._record(rb, wb, comp)
        return comp

    def final_wait(self, e="sp"):
        deps = [(k, 16 * n) for k, n in self.dcnt.items()]
        deps += [(k, self.cnt[k]) for k in self.COMPUTE if self.cnt[k] > 0 and k != e]
        self._emit_waits(e, deps)

    def emit(self, es):
        nc = self.nc
        for k in sorted(self.semkeys, key=str):
            self.sems[k] = es.enter_context(nc.semaphore("s_" + str(k)))
        block = es.enter_context(nc.Block())

        def make(e):
            def body(eng):
                for item in self.prog[e]:
                    if item[0] == "wait":
                        eng.wait_ge(self.sems[item[1]], item[2])
                    elif item[0] == "op":
                        ins = item[1](eng)
                        if item[2] is not None:
                            ins.then_inc(self.sems[item[2]], 1)
                    else:
                        item[1](eng).then_inc(self.sems[item[2]], 16)
            return body
        for e, reg in (("sp", block.sync), ("act", block.scalar), ("dve", block.vector),
                       ("pool", block.gpsimd), ("pe", block.tensor)):
            if self.prog[e]:
                reg(make(e))


class _Stop(Exception):
    pass


def build(S_LEN, dbg=False, stage=99):
    NT = S_LEN // TT
    NKT = S_LEN // 128
    NB = S_LEN // 256
    nc = bass.Bass("TRN2", target_bir_lowering=False)

    def din(name, shape):
        return nc.dram_tensor(name, shape, F32, kind="ExternalInput").ap()

    x = din("x", [S_LEN, D])
    vecs = din("vecs", [72, 128])
    bada = din("bada", [72, 128])
    w_ada = din("w_ada", [D, 9 * D])
    f1g, f1u, f1d = din("f1g", [D, FF]), din("f1u", [D, FF]), din("f1d", [FF, D])
    f2g, f2u, f2d = din("f2g", [D, FF]), din("f2u", [D, FF]), din("f2d", [FF, D])
    w_in = din("w_in", [D, 2560])
    w_out = din("w_out", [D, D])
    wqkv = din("wqkv", [3, 4, 128, 128])
    w_if = din("w_if", [1536, 8])
    b_if = din("b_if", [8])
    y = nc.dram_tensor("y", [S_LEN, D], F32, kind="ExternalOutput").ap()

    es = ExitStack()
    with es:
        S = Sched(nc)

        def sb(name, shape, dt=F32):
            return es.enter_context(nc.sbuf_tensor(name, shape, dt))

        ident_f = sb("ident_f", [128, 128])
        ones_m = sb("ones_m", [128, 128], BF16)
        ones_f = sb("ones_f", [128, 128])
        tri_f = sb("tri_f", [128, 128])
        causal4 = sb("causal4", [128, 4, 512], BF16)
        ident_b = sb("ident_b", [128, 128], BF16)
        sel_f = sb("sel_f", [128, 128])
        lr_all = sb("lr_all", [128, 2, TT])
        E01 = sb("E01", [128, 32])
        ELB = sb("ELB", [128, 32])
        OWN = sb("OWN", [128, 32])
        vecT = sb("vecT", [128, 72])
        modT = sb("modT", [128, 72])
        prm = sb("prm", [128, 9, 8])
        modrow = sb("modrow", [1, 2, 256])
        wqkv_b = sb("wqkv_b", [128, 12, 128], BF16)
        wif_sb = sb("wif_sb", [128, 12, 8])
        wg_b = sb("wg_b", [128, 8, 8], BF16)
        bif_bc = sb("bif_bc", [128, 8])
        Kc = sb("Kc", [128, 4, S_LEN], BF16)
        Vc = sb("Vc", [128, NKT, 8, 65], BF16)
        km_f = sb("km_f", [128, 4, 16])
        km_hi = sb("km_hi", [128, 4, 16], BF16)
        km_lo = sb("km_lo", [128, 4, 16], BF16)
        Cst = sb("Cst", [128, 4, 129])
        Cb = sb("Cb", [128, 4, 129], BF16)
        xm_halo = sb("xm_halo", [128, 4, 3], BF16)
        xT = sb("xT", [128, 8, TT])
        hT = sb("hT", [128, 8, TT], BF16)
        tmpf = sb("tmpf", [128, 2, TT])
        rstd = sb("rstd", [128, 2, TT])
        ring = sb("ring", [128, NR * 2048], BF16)
        UB = 66 * 1024
        U = sb("U", [128, UB // 2], BF16)
        Uf = U.bitcast(F32)
        ringf = ring.bitcast(F32)
        stg = ringf[:, 0:1536].rearrange("p (a e) -> p a e", a=12)
        wqkvT = ringf[:, 1536:3072].rearrange("p (a e) -> p a e", a=12)
        PS = [es.enter_context(nc.psum_tensor(f"ps{i}", [128, 512], F32)) for i in range(8)]

        class Carver:
            def __init__(self):
                self.off = 0

            def b(self, n):
                o = self.off
                self.off += -(-n * 2 // 64) * 64
                assert self.off <= UB, self.off
                return U[:, o // 2:o // 2 + n]

            def f(self, n):
                o = self.off
                self.off += -(-n * 4 // 64) * 64
                assert self.off <= UB, self.off
                return Uf[:, o // 4:o // 4 + n]

        cv = Carver()
        act = cv.b(NFF * TT).rearrange("p (j t) -> p j t", j=NFF)
        x_tm = cv.f(4 * D).rearrange("p (s d) -> p s d", s=4)
        cv_mark = cv.off
        cv.off = 0
        sq = cv.b(8 * TT).rearrange("p (c t) -> p c t", c=8)
        cv.off = 0
        PT = cv.b(3 * TT).rearrange("p (c t) -> p c t", c=3)
        biasT = cv.b(TT)
        xm_b = cv.b(4 * 515).rearrange("p (c t) -> p c t", c=4)
        Qz = cv.b(8 * TT).rearrange("p (c t) -> p c t", c=8)
        sz = cv.b(4 * TT).rearrange("p (c t) -> p c t", c=4)
        xc_b = cv.b(4 * TT).rearrange("p (c t) -> p c t", c=4)
        xcs = cv.b(4 * TT).rearrange("p (c t) -> p c t", c=4)
        biasq = cv.f(4 * 128).rearrange("p (s h k) -> p s h k", s=4, h=8)
        gm = cv.f(128).rearrange("p (h k) -> p h k", h=8)
        sel = cv.f(128).rearrange("p (h k) -> p h k", h=8)
        thr = cv.f(64).rearrange("p (h k) -> p h k", h=8)
        convacc = cv.f(TT)
        att_f = cv.f(4 * TT).rearrange("p (c t) -> p c t", c=4)
        o_sb = [cv.f(TT), cv.f(TT)]
        lr_f = [lr_all[:, 0, :], lr_all[:, 1, :]]
        QtS = [cv.b(512).rearrange("p (h t) -> p h t", h=4) for _ in range(2)]
        KtS = [cv.b(512).rearrange("p (h t) -> p h t", h=4)] * 2
        Ktok = [cv.b(512).rearrange("p (h t) -> p h t", h=4)] * 2
        Vp = [cv.b(4 * 129).rearrange("p (h t) -> p h t", h=4)] * 2
        Sc = [cv.b(512).rearrange("p (h t) -> p h t", h=4)] * 2
        hn = [cv.f(512).rearrange("p (h t) -> p h t", h=4)] * 2
        gsm = [cv.f(64), cv.f(64)]
        bst = cv.f(4 * 6).rearrange("p (h k) -> p h k", h=4)
        mv = cv.f(8).rearrange("p (h k) -> p h k", h=4)
        ctmp = cv.f(4 * 129).rearrange("p (h t) -> p h t", h=4)
        o_cat = cv.b(8 * TT).rearrange("p (c t) -> p c t", c=8)
        wa_st = x_tm.rearrange("p s (k n) -> p (s k) n", n=256)
        wa_bufs = [wa_st[:, 0:8, :], wa_st[:, 8:16, :]]

        def mm(out, lhsT, rhs, start, stop, inc=None):
            inc = stop if inc is None else inc
            S.op("pe", lambda e: e.matmul(out, lhsT=lhsT, rhs=rhs, start=start, stop=stop),
                 reads=[lhsT, rhs], writes=[out], inc=inc)

        def tr(out, in_, ident, inc=True):
            S.op("pe", lambda e: e.transpose(out=out, in_=in_, identity=ident),
                 reads=[in_, ident], writes=[out], inc=inc)

        def actf(out, in_, func, bias=0.0, scale=1.0, extra_reads=()):
            S.op("act", lambda e: e.activation(out=out, in_=in_, func=func, bias=bias, scale=scale),
                 reads=[in_] + list(extra_reads), writes=[out])

        def acopy(out, in_):
            S.op("act", lambda e: e.copy(out=out, in_=in_), reads=[in_], writes=[out])

        def vcopy(out, in_):
            S.op("dve", lambda e: e.tensor_copy(out=out, in_=in_), reads=[in_], writes=[out])

        def vtt(out, in0, in1, op):
            S.op("dve", lambda e: e.tensor_tensor(out=out, in0=in0, in1=in1, op=op), reads=[in0, in1], writes=[out])

        def vts(out, in0, s1, s2, op0, op1=None, rd=()):
            if op1 is None:
                S.op("dve", lambda e: e.tensor_scalar(out=out, in0=in0, scalar1=s1, scalar2=None, op0=op0),
                     reads=[in0] + list(rd), writes=[out])
            else:
                S.op("dve", lambda e: e.tensor_scalar(out=out, in0=in0, scalar1=s1, scalar2=s2, op0=op0, op1=op1),
                     reads=[in0] + list(rd), writes=[out])

        def vstt(out, in0, scalar, in1, op0, op1, rd=()):
            S.op("dve", lambda e: e.scalar_tensor_tensor(out=out, in0=in0, scalar=scalar, in1=in1, op0=op0, op1=op1),
                 reads=[in0, in1] + list(rd), writes=[out])

        def pmemset(ap, v):
            S.op("pool", lambda e: e.memset(ap, v), writes=[ap])

        def vmemset(ap, v):
            S.op("dve", lambda e: e.memset(ap, v), writes=[ap])

        def pasel(ap, pattern, cmp, cm, base=0):
            S.op("pool", lambda e: e.affine_select(out=ap, in_=ap, pattern=pattern, compare_op=cmp, fill=0.0,
                                                   base=base, channel_multiplier=cm), reads=[ap], writes=[ap])

        dumps = {}

        def chk(st):
            if stage <= st:
                raise _Stop()

        def dump(name, ap, dt=F32):
            if not dbg or name in dumps:
                return
            shp = list(ap.shape)
            dumps[name] = nc.dram_tensor("dbg_" + name, shp, dt, kind="ExternalOutput").ap()
            S.dma("sp", dumps[name], ap, "dbg_" + name)

        pmemset(ident_f[:], 1.0)
        pasel(ident_f[:], [[-1, 128]], ALU.is_equal, 1)
        pmemset(ones_m[:], 1.0 / D)
        pmemset(ones_f[:], 1.0)
        pmemset(tri_f[:], 1.0)
        pasel(tri_f[:], [[1, 128]], ALU.is_ge, -1)
        pmemset(causal4[:], 1.0)
        pasel(causal4[:], [[-128, 4], [1, 512]], ALU.is_ge, -1)
        pmemset(ident_b[:], 1.0)
        pasel(ident_b[:], [[-1, 128]], ALU.is_equal, 1)
        pmemset(sel_f[:], 1.0)
        pasel(sel_f[:], [[0, 128]], ALU.is_equal, 1, base=-64)
        pmemset(lr_all[:], 0.0)
        pmemset(E01[:, 0:16], 1.0)
        pmemset(E01[:, 16:32], 0.0)
        pmemset(ELB[:, 0:16], 0.0)
        pmemset(ELB[:, 16:32], -1e30)
        pmemset(OWN[:], 0.0)
        pmemset(OWN[:, 16:17], 1.0)
        pmemset(Vc[:, :, :, 64:65], 1.0)
        pmemset(km_f[:], 0.0)
        pmemset(km_hi[:], 0.0)
        pmemset(km_lo[:], 0.0)
        pmemset(Cst[:], 0.0)
        pmemset(Cb[:], 0.0)
        pmemset(xm_halo[:], 0.0)

        S.dma("sp", stg[0:72, 0, :], vecs, "ldv1")
        tr(PS[0][:, 0:72], stg[0:72, 0, :], ident_f[0:72, 0:72])
        vcopy(vecT[:], PS[0][:, 0:72])
        S.dma("sp", stg[0:72, 1, :], bada, "ldv2")
        tr(PS[1][:, 0:72], stg[0:72, 1, :], ident_f[0:72, 0:72])
        vcopy(modT[:], PS[1][:, 0:72])
        S.dma("sp", bif_bc[:], b_if.partition_broadcast(128), "ldv3")
        S.dma("sp", wif_sb[:], w_if.rearrange("(j p) g -> p j g", p=128), "ldv4")

        NG = 36
        for g in range(NG):
            buf = wa_bufs[g % 2]
            S.dma("sp", buf, w_ada[:, g * 256:(g + 1) * 256].rearrange("(kc p) n -> p kc n", p=128), f"lda{g % 2}")
            pso = PS[2 + g % 2]
            for kc in range(8):
                mm(pso[0:1, 0:256], vecT[:, kc:kc + 1], buf[:, kc, :], kc == 0, kc == 7)
            acopy(modrow[0:1, g % 2, :], pso[0:1, 0:256])
            for jj in range(2):
                j = 2 * g + jj
                mm(PS[4][:, j:j + 1], modrow[0:1, g % 2, jj * 128:(jj + 1) * 128], ones_f[0:1, 0:1], True, True)
        vtt(modT[:], modT[:], PS[4][:, 0:72], ALU.add)

        def modv(i):
            return modT[:, i * 8:(i + 1) * 8]

        def vrow(r, n=8):
            return vecT[:, r:r + n]
        for blk, (nrow, half) in enumerate(((8, 0.5), (16, 1.0), (24, 0.5))):
            vstt(prm[:, 3 * blk + 0, :], modv(3 * blk + 1), 1.0, vrow(nrow), ALU.add, ALU.mult)
            vcopy(prm[:, 3 * blk + 1, :], modv(3 * blk + 0))
            vts(prm[:, 3 * blk + 2, :], modv(3 * blk + 2), 1.0, half, ALU.add, ALU.mult)
        fin_g = vrow(32)
        attn_g = vrow(40, 4)
        mn_g = vrow(44, 4)
        skip_g = vrow(48, 4)
        conv_bv = vrow(52, 4)

        def conv_wv(j, hc):
            return vecT[:, 56 + j * 4 + hc:57 + j * 4 + hc]

        S.dma("sp", stg[:], wqkv.rearrange("a h d e -> d (a h) e"), "ldv5")
        vcopy(wqkv_b[:], stg[:])
        for i in range(12):
            tr(PS[5 + i % 2][:, 0:128], stg[:, i, :], ident_f[:])
            acopy(wqkvT[:, i, :], PS[5 + i % 2][:, 0:128])
        for h in range(4):
            mm(PS[7][:, h * 8:(h + 1) * 8], wqkvT[:, h, :], wif_sb[:, 3 * h, :], True, False)
            mm(PS[7][:, h * 8:(h + 1) * 8], wqkvT[:, 4 + h, :], wif_sb[:, 3 * h + 1, :], False, True, inc=False)
            mm(PS[7][:, (4 + h) * 8:(5 + h) * 8], wqkvT[:, 8 + h, :], wif_sb[:, 3 * h + 2, :], True, True, inc=(h == 3))
        vcopy(wg_b[:].rearrange("p a g -> p (a g)"), PS[7][:, 0:64])

        units = []

        def add_unit_cols(w, c0):
            units.append(w[:, c0:c0 + 256].rearrange("(kc p) n -> p kc n", p=128))

        def add_unit_rows(w, r0):
            units.append(w[r0:r0 + 256, :].rearrange("(jj p) n -> p jj n", p=128))

        for t in range(NT):
            for g in range(11):
                add_unit_cols(f1g, g * 256)
                add_unit_cols(f1u, g * 256)
            for g in range(11):
                add_unit_rows(f1d, g * 256)
            for g in range(10):
                add_unit_cols(w_in, g * 256)
            for g in range(4):
                add_unit_cols(w_out, g * 256)
            for g in range(11):
                add_unit_cols(f2g, g * 256)
                add_unit_cols(f2u, g * 256)
            for g in range(11):
                add_unit_rows(f2d, g * 256)
        wstate = {"issued": 0, "next": 0}

        def wview(u, rows):
            slot = ring[:, (u % NR) * 2048:(u % NR + 1) * 2048]
            if rows:
                return slot.rearrange("p (jj n) -> p jj n", jj=2)
            return slot.rearrange("p (kc n) -> p kc n", kc=8)

        def wissue_upto(n):
            while wstate["issued"] < min(n, len(units)):
                u = wstate["issued"]
                src = units[u]
                rows = (src.shape[1] == 2)
                S.dma("pool", wview(u, rows), src, f"w{u % NR}")
                wstate["issued"] += 1

        def wnext(rows=False):
            u = wstate["next"]
            wstate["next"] += 1
            wissue_upto(u + NR - 1)
            return wview(u, rows)

        def rms_mod(ns, sh):
            for c in range(8):
                actf(sq[:, c, :], xT[:, c, :], AF.Square)
            for c in range(8):
                mm(PS[6][:], ones_m[:], sq[:, c, :], c == 0, c == 7)
            actf(rstd[:, 0, :], PS[6][:], AF.Sqrt, bias=eps_t[:, 0:1], scale=1.0, extra_reads=[eps_t[:, 0:1]])
            S.op("dve", lambda e: e.reciprocal(out=rstd[:, 1, :], in_=rstd[:, 0, :]),
                 reads=[rstd[:, 0, :]], writes=[rstd[:, 1, :]])
            for c in range(8):
                vstt(tmpf[:, c % 2, :], xT[:, c, :], ns[:, c:c + 1], rstd[:, 1, :], ALU.mult, ALU.mult,
                     rd=[ns[:, c:c + 1]])
                actf(hT[:, c, :], tmpf[:, c % 2, :], AF.Identity, bias=sh[:, c:c + 1], scale=1.0,
                     extra_reads=[sh[:, c:c + 1]])

        def ffn(rg):
            for g in range(11):
                wg_v = wnext()
                wu_v = wnext()
                for jj in range(2):
                    j = 2 * g + jj
                    pg, pu = PS[j % 2], PS[2 + j % 2]
                    for kc in range(8):
                        mm(pg[:], wg_v[:, kc, jj * 128:(jj + 1) * 128], hT[:, kc, :], kc == 0, kc == 7)
                    for kc in range(8):
                        mm(pu[:], wu_v[:, kc, jj * 128:(jj + 1) * 128], hT[:, kc, :], kc == 0, kc == 7)
                    actf(tmpf[:, j % 2, :], pg[:], AF.Silu)
                    vtt(act[:, j, :], tmpf[:, j % 2, :], pu[:], ALU.mult)
            dump('act', act[:], BF16)
            for g in range(11):
                wd_v = wnext(rows=True)
                for jj in range(2):
                    j = 2 * g + jj
                    for i in range(8):
                        mm(PS[i][:], wd_v[:, jj, i * 128:(i + 1) * 128], act[:, j, :], j == 0, j == NFF - 1,
                           inc=(i == 7 and jj == 1))
            for i in range(8):
                vstt(xT[:, i, :], PS[i][:], rg[:, i:i + 1], xT[:, i, :], ALU.mult, ALU.add, rd=[rg[:, i:i + 1]])

        eps_t = sb("eps_t", [128, 1])
        pmemset(eps_t[:], EPS)

        def mixer(t):
            c0 = t * TT
            for hh in range(8):
                z0 = 64 * (1 - hh % 2)
                vmemset(Qz[z0:z0 + 64, hh, :], 0.0)
            for g in range(4):
                wv_ = wnext()
                for jj in range(2):
                    oc = 2 * g + jj
                    ps = PS[4 + oc % 2]
                    for kc in range(8):
                        mm(ps[:], wv_[:, kc, jj * 128:(jj + 1) * 128], hT[:, kc, :], kc == 0, kc == 7)
                    if oc < 4:
                        acopy(Qz[0:64, 2 * oc, :], ps[0:64, :])
                        acopy(Qz[64:128, 2 * oc + 1, :], ps[64:128, :])
                    else:
                        vcopy(Kc[:, oc - 4, c0:c0 + TT], ps[:])
            for g in range(2):
                wv_ = wnext()
                for s4 in range(4):
                    ps = PS[6 + s4 % 2]
                    for kc in range(8):
                        mm(ps[:, 0:256], hT[:, kc, s4 * 128:(s4 + 1) * 128], wv_[:, kc, :], kc == 0, kc == 7)
                    dst = Vc[:, t * 4 + s4, g * 4:(g + 1) * 4, 0:64]
                    src = ps[:, 0:256].rearrange("p (h e) -> p h e", h=4)
                    if s4 % 2 == 0:
                        acopy(dst, src)
                    else:
                        vcopy(dst, src)
            vcopy(xm_b[:, :, 0:3], xm_halo[:])
            for g in range(2):
                wv_ = wnext()
                for jj in range(2):
                    oc = 2 * g + jj
                    ps = PS[4 + oc % 2]
                    for kc in range(8):
                        mm(ps[:], wv_[:, kc, jj * 128:(jj + 1) * 128], hT[:, kc, :], kc == 0, kc == 7)
                    acopy(xm_b[:, oc, 3:515], ps[:])
            vcopy(xm_halo[:], xm_b[:, :, 512:515])
            for g in range(2):
                wv_ = wnext()
                for jj in range(2):
                    oc = 2 * g + jj
                    ps = PS[6 + oc % 2]
                    for kc in range(8):
                        mm(ps[:], wv_[:, kc, jj * 128:(jj + 1) * 128], hT[:, kc, :], kc == 0, kc == 7)
                    actf(sz[:, oc, :], ps[:], AF.Silu)
            for hc in range(4):
                vts(convacc, xm_b[:, hc, 0:512], conv_wv(0, hc), conv_bv[:, hc:hc + 1], ALU.mult, ALU.add,
                    rd=[conv_wv(0, hc), conv_bv[:, hc:hc + 1]])
                for j in range(1, 4):
                    vstt(convacc, xm_b[:, hc, j:j + 512], conv_wv(j, hc), convacc, ALU.mult, ALU.add,
                         rd=[conv_wv(j, hc)])
                actf(xc_b[:, hc, :], convacc, AF.Silu)
                vts(xcs[:, hc, :], xc_b[:, hc, :], skip_g[:, hc:hc + 1], None, ALU.mult, rd=[skip_g[:, hc:hc + 1]])
            dump('xm_b', xm_b[:], BF16); dump('sz', sz[:], BF16); dump('xc_b', xc_b[:], BF16)
            chk(3)
            attention(t, None)
            chk(4)
            dump('att_f', att_f[:]); dump('biasq', biasq[:])
            gens = [mlstm_chunk(ch, PS[2 * (ch % 2)], PS[2 * (ch % 2) + 1]) for ch in range(4)]
            LAG = 7
            active, nstep, nxt = [], {}, 0
            while active or nxt < 4:
                if nxt < 4 and (not active or nstep[active[-1]] >= LAG) and len(active) < 2:
                    active.append(nxt)
                    nstep[nxt] = 0
                    nxt += 1
                for g in list(active):
                    try:
                        next(gens[g])
                        nstep[g] += 1
                    except StopIteration:
                        active.remove(g)
            chk(5)
            dump('o_cat', o_cat[:], BF16)
            rg = prm[:, 5, :]
            for g in range(4):
                wv_ = wnext()
                for jj in range(2):
                    oc = 2 * g + jj
                    ps = PS[4 + oc % 2]
                    for kc in range(8):
                        mm(ps[:], wv_[:, kc, jj * 128:(jj + 1) * 128], o_cat[:, kc, :], kc == 0, kc == 7)
                    vstt(xT[:, oc, :], ps[:], rg[:, oc:oc + 1], xT[:, oc, :], ALU.mult, ALU.add, rd=[rg[:, oc:oc + 1]])

        def attention(t, mgen=None):
            c0 = t * TT
            for c in range(4):
                for b in range(2):
                    blk = 2 * t + b
                    src = Kc[:, c, blk * 256:(blk + 1) * 256]
                    S.op("dve", lambda e, src=src, c=c, blk=blk: e.tensor_reduce(
                        out=km_f[:, c, blk:blk + 1], in_=src, axis=AX.X, op=ALU.add),
                        reads=[src], writes=[km_f[:, c, blk:blk + 1]])
                kk = km_f[:, c, 2 * t:2 * t + 2]
                vts(kk, kk, 1.0 / 256, None, ALU.mult)
                vcopy(km_hi[:, c, 2 * t:2 * t + 2], kk)
                vtt(kk, kk, km_hi[:, c, 2 * t:2 * t + 2], ALU.subtract)
                vcopy(km_lo[:, c, 2 * t:2 * t + 2], kk)
            chk(3.1)
            for s4 in range(4):
                qb = 2 * t + s4 // 2
                for h in range(8):
                    c, r0 = h // 2, 64 * (h % 2)
                    psG = PS[6 + h % 2]
                    qsl = Qz[:, h, s4 * 128:(s4 + 1) * 128]
                    mm(psG[:, c * 16:(c + 1) * 16], qsl, km_hi[:, c, :], True, False)
                    mm(psG[:, c * 16:(c + 1) * 16], qsl, km_lo[:, c, :], False, True, inc=(h >= 6))
                chk(3.11)
                elb = ELB[:, 16 - qb:32 - qb].unsqueeze(1).broadcast_to([128, 8, 16])
                e01 = E01[:, 16 - qb:32 - qb].unsqueeze(1).broadcast_to([128, 8, 16])
                own = OWN[:, 16 - qb:32 - qb].unsqueeze(1).broadcast_to([128, 8, 16])
                gm4 = gm.rearrange("p (c two) k -> p c two k", two=2)
                for par in range(2):
                    vtt(gm4[:, :, par, :], PS[6 + par][:, 0:64].rearrange("p (c k) -> p c k", c=4),
                        ELB[:, 16 - qb:32 - qb].unsqueeze(1).broadcast_to([128, 4, 16]), ALU.add)
                chk(3.12)
                for h in range(8):
                    S.op("dve", lambda e, h=h: e.max(out=thr[:, h, :], in_=gm[:, h, :]),
                         reads=[gm[:, h, :]], writes=[thr[:, h, :]])
                chk(3.13)
                vtt(sel, gm, thr[:, :, 2:3].broadcast_to([128, 8, 16]), ALU.is_ge)
                chk(3.14)
                vtt(sel, sel, e01, ALU.mult)
                vtt(sel, sel, own, ALU.add)
                vts(biasq[:, s4, :, :], sel, 1.0, BIG, ALU.subtract, ALU.mult)
                chk(3.15)
                if s4 == 1:
                    chk(3.16)
            chk(3.2)
            for s4 in range(4):
                tr(PS[7][:, s4 * 128:(s4 + 1) * 128], biasq[:, s4, :, :].rearrange("p h k -> p (h k)"), ident_f[:],
                   inc=(s4 == 3))
            vcopy(biasT, PS[7][:])
            nkt = 4 * (t + 1)
            LA = 2
            pend = None
            for h in range(8):
                c, r0 = h // 2, 64 * (h % 2)
                pso = PS[3 + h % 2]

                def qk(kt, c=c, h=h):
                    pss = PS[kt % 3]
                    j = 16 * h + kt // 2
                    mm(pss[:], Kc[:, c, kt * 128:(kt + 1) * 128], Qz[:, h, :], True, False)
                    mm(pss[:], ident_b[:, j:j + 1].broadcast_to([128, 128]), biasT, False, True)

                for kt in range(min(LA, nkt)):
                    qk(kt)
                if pend is not None:
                    pend()
                    pend = None
                for kt in range(nkt):
                    if kt + LA < nkt:
                        qk(kt + LA)
                    pss = PS[kt % 3]
                    pt = PT[:, kt % 3, :]
                    actf(pt, pss[:], AF.Exp, scale=0.125)
                    if kt >= 4 * t:
                        vtt(pt, pt, causal4[:, kt - 4 * t, :], ALU.mult)
                    mm(pso[0:65, :], Vc[:, kt, h, :], pt, kt == 0, kt == nkt - 1, inc=True)
                    if mgen is not None:
                        next(mgen, None)
                lr, osb, psb = lr_f[h % 2], o_sb[h % 2], PS[5 + h % 2]
                actf(lr[32:33, :], pso[64:65, :], AF.Ln)
                actf(lr[64:65, :], lr[32:33, :], AF.Exp, scale=-1.0)
                acopy(osb[0:64, :], pso[0:64, :])

                def ep(lr=lr, osb=osb, psb=psb, c=c, r0=r0):
                    mm(psb[:], sel_f[:], lr, True, True)
                    vtt(att_f[r0:r0 + 64, c, :], osb[0:64, :], psb[0:64, :], ALU.mult)
                pend = ep
            pend()
            chk(3.6)
            for c in range(4):
                actf(sq[:, c, :], att_f[:, c, :], AF.Square, scale=2.0 ** 0.5)
            for c in range(4):
                mm(PS[5][:], ones_m[:], sq[:, c, :], c == 0, c == 3)
            actf(rstd[:, 0, :], PS[5][:], AF.Sqrt, bias=eps_t[:, 0:1], scale=1.0, extra_reads=[eps_t[:, 0:1]])
            S.op("dve", lambda e: e.reciprocal(out=rstd[:, 1, :], in_=rstd[:, 0, :]),
                 reads=[rstd[:, 0, :]], writes=[rstd[:, 1, :]])
            for c in range(4):
                vstt(o_cat[:, c, :], att_f[:, c, :], attn_g[:, c:c + 1], rstd[:, 1, :], ALU.mult, ALU.mult,
                     rd=[attn_g[:, c:c + 1]])

        def mlstm_chunk(ch, A, Bk):
            kscale = 128.0 ** -0.5
            if True:
                cs = slice(ch * 128, (ch + 1) * 128)
                cs3 = slice(3 + ch * 128, 3 + (ch + 1) * 128)
                b2 = ch % 2
                gs = gsm[b2]
                for h in range(4):
                    mm(A[:, 0:8], xc_b[:, h, cs], wg_b[:, h, :], h == 0, False)
                for h in range(4):
                    mm(A[:, 0:8], xm_b[:, h, cs3], wg_b[:, 4 + h, :], False, h == 3)
                vtt(gs[:, 0:8], A[:, 0:8], bif_bc[:], ALU.add)
                actf(gs[:, 28:32], gs[:, 4:8], AF.Exp, scale=-1.0)
                actf(gs[:, 4:8], gs[:, 28:32], AF.Ln, bias=1.0)
                vts(gs[:, 4:8], gs[:, 4:8], -1.0, None, ALU.mult)
                yield
                yield
                mm(A[:, 16:20], tri_f[:], gs[:, 4:8], True, True)
                mm(A[:, 24:28], ones_f[:], gs[:, 4:8], True, True)
                vcopy(gs[:, 8:16].rearrange("p (a k) -> p a k", a=2),
                      A[:, 16:32].rearrange("p (a k) -> p a k", a=2)[:, :, 0:4])
                vtt(gs[:, 28:32], gs[:, 0:4], gs[:, 8:12], ALU.subtract)
                actf(gs[:, 16:20], gs[:, 28:32], AF.Exp)
                actf(gs[:, 20:28], gs[:, 8:16], AF.Exp)
                yield
                for h in range(4):
                    mm(Bk[:, h * 128:(h + 1) * 128], wqkv_b[:, h, :], xc_b[:, h, cs], True, True, inc=(h == 3))
                vcopy(QtS[b2].rearrange("p h t -> p (h t)"), Bk[:])
                yield
                for h in range(4):
                    mm(A[:, h * 128:(h + 1) * 128], wqkv_b[:, 4 + h, :], xc_b[:, h, cs], True, True, inc=(h == 3))
                vts(KtS[b2].rearrange("p h t -> p (h t)"), A[:], kscale, None, ALU.mult)
                yield
                for h in range(4):
                    mm(Bk[:, h * 128:(h + 1) * 128], xc_b[:, h, cs], wqkv_b[:, 4 + h, :], True, True, inc=(h == 3))
                vts(Ktok[b2].rearrange("p h t -> p (h t)"), Bk[:], kscale, None, ALU.mult)
                yield
                for h in range(4):
                    mm(A[:, h * 128:(h + 1) * 128], xm_b[:, h, cs3], wqkv_b[:, 8 + h, :], True, True, inc=(h == 3))
                a_bc = gs[:, 16:20].unsqueeze(2).broadcast_to([128, 4, 128])
                vtt(Vp[b2][:, :, 0:128], A[:].rearrange("p (h e) -> p h e", h=4), a_bc, ALU.mult)
                vcopy(Vp[b2][:, :, 128:129], gs[:, 16:20].unsqueeze(2))
                yield
                for h in range(4):
                    mm(Bk[:, h * 128:(h + 1) * 128], KtS[b2][:, h, :], QtS[b2][:, h, :], True, True, inc=(h == 3))
                vtt(Sc[b2][:], Bk[:].rearrange("p (h t) -> p h t", h=4),
                    tri_f[:].unsqueeze(1).broadcast_to([128, 4, 128]), ALU.mult)
                yield
                pU = [A, Bk]
                for h in range(4):
                    o = pU[h // 2][:, (h % 2) * 129:(h % 2) * 129 + 129]
                    mm(o, Ktok[b2][:, h, :], Vp[b2][:, h, :], True, True, inc=(h % 2 == 1))
                for hp in range(2):
                    vtt(ctmp[:, 2 * hp:2 * hp + 2, :], Cst[:, 2 * hp:2 * hp + 2, :],
                        pU[hp][:, 0:258].rearrange("p (h v) -> p h v", h=2), ALU.add)
                yield
                pH = [A, Bk]
                for h in range(4):
                    o = pH[h // 2][:, (h % 2) * 129:(h % 2) * 129 + 129]
                    mm(o, Sc[b2][:, h, :], Vp[b2][:, h, :], True, False)
                    mm(o, QtS[b2][:, h, :], Cb[:, h, :], False, True, inc=(h % 2 == 1))
                vtt(Cst[:], ctmp[:], gs[:, 24:28].unsqueeze(2).broadcast_to([128, 4, 129]), ALU.mult)
                vcopy(Cb[:], Cst[:])
                for h in range(4):
                    hp, hh = h // 2, h % 2
                    S.op("dve", lambda e, hp=hp, hh=hh, h=h: e.bn_stats(out=bst[:, h, :], in_=pH[hp][:, hh * 129:hh * 129 + 128]),
                         reads=[pH[hp][:, hh * 129:hh * 129 + 128]], writes=[bst[:, h, :]])
                for hp in range(2):
                    Hv = pH[hp][:, 0:258].rearrange("p (h v) -> p h v", h=2)
                    vtt(gs[:, 28 + 2 * hp:30 + 2 * hp].unsqueeze(2), Hv[:, :, 128:129],
                        gs[:, 20 + 2 * hp:22 + 2 * hp].unsqueeze(2), ALU.mult)
                for h in range(4):
                    S.op("dve", lambda e, h=h: e.bn_aggr(out=mv[:, h, :], in_=bst[:, h, :]),
                         reads=[bst[:, h, :]], writes=[mv[:, h, :]])
                vts(gs[:, 52:56], gs[:, 28:32], -1.0, None, ALU.mult)
                vtt(gs[:, 32:36], gs[:, 28:32], gs[:, 52:56], ALU.max)
                vts(gs[:, 32:36], gs[:, 32:36], 1.0, None, ALU.max)
                S.op("dve", lambda e, gs=gs: e.reciprocal(out=gs[:, 36:40], in_=gs[:, 32:36]),
                     reads=[gs[:, 32:36]], writes=[gs[:, 36:40]])
                vtt(gs[:, 36:40], gs[:, 36:40], gs[:, 20:24], ALU.mult)
                vtt(gs[:, 40:44], gs[:, 36:40], gs[:, 36:40], ALU.mult)
                vtt(gs[:, 40:44].unsqueeze(2), gs[:, 40:44].unsqueeze(2), mv[:, :, 1:2], ALU.mult)
                actf(gs[:, 44:48], gs[:, 40:44], AF.Sqrt, bias=eps_t[:, 0:1], scale=1.0, extra_reads=[eps_t[:, 0:1]])
                S.op("dve", lambda e, gs=gs: e.reciprocal(out=gs[:, 48:52], in_=gs[:, 44:48]),
                     reads=[gs[:, 44:48]], writes=[gs[:, 48:52]])
                vtt(gs[:, 48:52], gs[:, 48:52], gs[:, 36:40], ALU.mult)
                for h in range(4):
                    hp, hh = h // 2, h % 2
                    vts(hn[b2][:, h, :], pH[hp][:, hh * 129:hh * 129 + 128], mv[:, h, 0:1], gs[:, 48 + h:49 + h],
                        ALU.subtract, ALU.mult, rd=[mv[:, h, 0:1], gs[:, 48 + h:49 + h]])
                yield
                yield
                yield
                for h in range(4):
                    tr(A[:, h * 128:(h + 1) * 128], hn[b2][:, h, :], ident_f[:], inc=(h == 3))
                for h in range(4):
                    vstt(tmpf[:, 0, h * 128:(h + 1) * 128], A[:, h * 128:(h + 1) * 128], mn_g[:, h:h + 1],
                         xcs[:, h, cs], ALU.mult, ALU.add, rd=[mn_g[:, h:h + 1]])
                    vtt(o_cat[:, 4 + h, cs], tmpf[:, 0, h * 128:(h + 1) * 128], sz[:, h, cs], ALU.mult)
                yield

        try:
          chk(0)
          for t in range(NT):
              c0 = t * TT
              S.dma("sp", x_tm, x[c0:c0 + TT, :].rearrange("(s p) d -> p s d", p=128), "ldx")
              for c in range(8):
                  ps = PS[6 + c % 2]
                  for s4 in range(4):
                      tr(ps[:, s4 * 128:(s4 + 1) * 128], x_tm[:, s4, c * 128:(c + 1) * 128], ident_f[:], inc=(s4 == 3))
                  acopy(xT[:, c, :], ps[:])
              dump('modT', modT[:]); dump('prm', prm[:]); dump('xT0', xT[:])
              rms_mod(prm[:, 0, :], prm[:, 1, :])
              dump('hT1', hT[:], BF16)
              chk(1)
              ffn(prm[:, 2, :])
              chk(2)
              dump('xT1', xT[:])
              rms_mod(prm[:, 3, :], prm[:, 4, :])
              dump('hT2', hT[:], BF16)
              mixer(t)
              dump('xT2', xT[:])
              rms_mod(prm[:, 6, :], prm[:, 7, :])
              ffn(prm[:, 8, :])
              for c in range(8):
                  actf(sq[:, c, :], xT[:, c, :], AF.Square)
              for c in range(8):
                  mm(PS[6][:], ones_m[:], sq[:, c, :], c == 0, c == 7)
              actf(rstd[:, 0, :], PS[6][:], AF.Sqrt, bias=eps_t[:, 0:1], scale=1.0, extra_reads=[eps_t[:, 0:1]])
              S.op("dve", lambda e: e.reciprocal(out=rstd[:, 1, :], in_=rstd[:, 0, :]),
                   reads=[rstd[:, 0, :]], writes=[rstd[:, 1, :]])
              for c in range(8):
                  vstt(tmpf[:, c % 2, :], xT[:, c, :], fin_g[:, c:c + 1], rstd[:, 1, :], ALU.mult, ALU.mult,
                       rd=[fin_g[:, c:c + 1]])
                  ps = PS[4 + c % 2]
                  for s4 in range(4):
                      tr(ps[:, s4 * 128:(s4 + 1) * 128], tmpf[:, c % 2, s4 * 128:(s4 + 1) * 128], ident_f[:], inc=(s4 == 3))
                  acopy(x_tm[:, :, c * 128:(c + 1) * 128], ps[:].rearrange("p (s d) -> p s d", s=4))
              S.dma("sp", y[c0:c0 + TT, :].rearrange("(s p) d -> p s d", p=128), x_tm, "sty")
        except _Stop:
            pass
        S.final_wait("sp")
        S.emit(es)
    return nc


def make_in_maps(S_LEN, x, c, w_ada, b_ada, ffn1_norm, ffn1_w_gate, ffn1_w_up, ffn1_w_down,
                 mix_norm, w_in, conv_w, conv_b, w_q_m, w_k_m, w_v_m, w_if, b_if,
                 mlstm_norm, mlstm_skip, attn_norm, w_out,
                 ffn2_norm, ffn2_w_gate, ffn2_w_up, ffn2_w_down, final_norm):
    f = lambda a: np.ascontiguousarray(np.asarray(a, dtype=np.float32))
    B = x.shape[0]
    shared = {
        "bada": f(b_ada[0]).reshape(72, 128),
        "w_ada": f(w_ada[0]),
        "f1g": f(ffn1_w_gate[0]), "f1u": f(ffn1_w_up[0]), "f1d": f(ffn1_w_down[0]),
        "f2g": f(ffn2_w_gate[0]), "f2u": f(ffn2_w_up[0]), "f2d": f(ffn2_w_down[0]),
        "w_in": f(w_in[0]), "w_out": f(w_out[0]),
        "wqkv": f(np.stack([w_q_m[0], w_k_m[0], w_v_m[0]])),
        "w_if": f(w_if[0]), "b_if": f(b_if[0]),
    }
    maps = []
    for b in range(B):
        rows = [f(c[b]).reshape(8, 128), f(ffn1_norm[0]).reshape(8, 128), f(mix_norm[0]).reshape(8, 128),
                f(ffn2_norm[0]).reshape(8, 128), f(final_norm).reshape(8, 128), f(attn_norm[0]).reshape(4, 128),
                f(mlstm_norm[0]).reshape(4, 128), f(mlstm_skip[0]).reshape(4, 128), f(conv_b[0]).reshape(4, 128),
                f(conv_w[0]).reshape(16, 128)]
        m = dict(shared)
        m["vecs"] = np.ascontiguousarray(np.concatenate(rows, axis=0))
        m["x"] = f(x[b])
        maps.append(m)
    return maps


def kernel(**inputs):
    x = np.asarray(inputs["x"])
    B, S_LEN, _ = x.shape
    nc = build(S_LEN)
    maps = make_in_maps(S_LEN, **inputs)
    res = run_bass_kernel_spmd(nc, maps, core_ids=list(range(B)))
    return np.stack([np.asarray(r["y"], dtype=np.float32) for r in res.results], axis=0)
```

```python
from contextlib import ExitStack
import numpy as np
import concourse.bass as bass
import concourse.mybir as mybir
from concourse.bass_utils import run_bass_kernel_spmd

F32 = mybir.dt.float32
BF16 = mybir.dt.bfloat16
ALU = mybir.AluOpType
AF = mybir.ActivationFunctionType
AX = mybir.AxisListType

D = 1024
FF = 2816
NFF = 22
TT = 512
EPS = 1e-6
NR = 6
BIG = 30000.0


class Sched:
    COMPUTE = ("pe", "act", "dve", "pool")

    def __init__(self, nc, self_sync=True):
        self.nc = nc
        self.self_sync = self_sync
        self.eng = {"pe": nc.tensor, "act": nc.scalar, "dve": nc.vector, "pool": nc.gpsimd, "sp": nc.sync}
        self.prog = {k: [] for k in self.eng}
        self.cnt = {k: 0 for k in self.COMPUTE}
        self.dcnt = {}
        self.waited = {k: {} for k in self.eng}
        self.acc = {}
        self.semkeys = set(self.COMPUTE)
        self.sems = {}

    @staticmethod
    def box(ap):
        t = ap.tensor
        name = t.name
        dims = list(ap.ap)
        esz = 2 if ap.dtype == BF16 else 4
        if type(t).__name__.startswith("DRam"):
            lo = ap.offset
            hi = lo + sum((c - 1) * abs(s) for s, c in dims) + 1
            return (name, 0, 1, lo * esz, hi * esz)
        pstride = dims[0][0]
        pcnt = dims[0][1]
        if pstride == 0:
            p0, f0 = 0, ap.offset
        else:
            p0 = ap.offset // pstride
            f0 = ap.offset - p0 * pstride
        ext = sum((c - 1) * abs(s) for s, c in dims[1:]) + 1
        lo, hi = f0 * esz, (f0 + ext) * esz
        if type(t).__name__.startswith("PSum"):
            return (name, 0, 128, (lo // 2048) * 2048, -(-hi // 2048) * 2048)
        return (name, p0, p0 + pcnt, lo, hi)

    @staticmethod
    def _ov(a, b):
        return a[1] < b[2] and b[1] < a[2] and a[3] < b[4] and b[3] < a[4]

    @staticmethod
    def _contains(a, b):
        return a[1] <= b[1] and b[2] <= a[2] and a[3] <= b[3] and b[4] <= a[4]

    def _deps(self, reads, writes):
        deps = []
        for b in reads:
            for rec in self.acc.get(b[0], ()):
                if rec[0] == "W" and self._ov(rec[1], b):
                    deps.append(rec[2])
        for b in writes:
            for rec in self.acc.get(b[0], ()):
                if self._ov(rec[1], b):
                    deps.append(rec[2])
        return deps

    def _record(self, reads, writes, comp):
        for b in writes:
            lst = self.acc.setdefault(b[0], [])
            lst[:] = [r for r in lst if not self._contains(b, r[1])]
            lst.append(("W", b, comp))
        for b in reads:
            lst = self.acc.setdefault(b[0], [])
            lst[:] = [r for r in lst if not (r[0] == "R" and r[2][0] == comp[0] and r[2][1] <= comp[1]
                                             and self._contains(b, r[1]))]
            lst.append(("R", b, comp))

    def _emit_waits(self, e, deps):
        best = {}
        for k, v in deps:
            if v > best.get(k, 0):
                best[k] = v
        for k, v in best.items():
            if k == e and (e == "pe" or not self.self_sync):
                continue
            if k == e and v > self.cnt[e]:
                continue
            if self.waited[e].get(k, 0) >= v:
                continue
            self.waited[e][k] = v
            self.prog[e].append(("wait", k, v))

    def op(self, e, fn, reads=(), writes=(), inc=True):
        rb = [self.box(a) for a in reads]
        wb = [self.box(a) for a in writes]
        self._emit_waits(e, self._deps(rb, wb))
        comp = (e, self.cnt[e] + 1)
        self.prog[e].append(("op", fn, e if inc else None))
        self._record(rb, wb, comp)
        if inc:
            self.cnt[e] += 1
        return comp

    def dma(self, q, out, in_, key, **kw):
        rb = [self.box(in_)]
        wb = [self.box(out)]
        self._emit_waits(q, self._deps(rb, wb))
        self.semkeys.add(key)
        n = self.dcnt.get(key, 0) + 1
        self.dcnt[key] = n
        comp = (key, 16 * n)

        def fn(eng, out=out, in_=in_, kw=kw):
            return eng.dma_start(out=out, in_=in_, **kw)
        self.prog[q].append(("dma", fn, key))
        self._record(rb, wb, comp)
        return comp

    def final_wait(self, e="sp"):
        deps = [(k, 16 * n) for k, n in self.dcnt.items()]
        deps += [(k, self.cnt[k]) for k in self.COMPUTE if self.cnt[k] > 0 and k != e]
        self._emit_waits(e, deps)

    def emit(self, es):
        nc = self.nc
        for k in sorted(self.semkeys, key=str):
            self.sems[k] = es.enter_context(nc.semaphore("s_" + str(k)))
        block = es.enter_context(nc.Block())

        def make(e):
            def body(eng):
                for item in self.prog[e]:
                    if item[0] == "wait":
                        eng.wait_ge(self.sems[item[1]], item[2])
                    elif item[0] == "op":
                        ins = item[1](eng)
                        if item[2] is not None:
                            ins.then_inc(self.sems[item[2]], 1)
                    else:
                        item[1](eng).then_inc(self.sems[item[2]], 16)
            return body
        for e, reg in (("sp", block.sync), ("act", block.scalar), ("dve", block.vector),
                       ("pool", block.gpsimd), ("pe", block.tensor)):
            if self.prog[e]:
                reg(make(e))


class _Stop(Exception):
    pass


def build(S_LEN, dbg=False, stage=99):
    NT = S_LEN // TT
    NKT = S_LEN // 128
    NB = S_LEN // 256
    nc = bass.Bass("TRN2", target_bir_lowering=False)

    def din(name, shape):
        return nc.dram_tensor(name, shape, F32, kind="ExternalInput").ap()

    x = din("x", [S_LEN, D])
    vecs = din("vecs", [72, 128])
    bada = din("bada", [72, 128])
    w_ada = din("w_ada", [D, 9 * D])
    f1g, f1u, f1d = din("f1g", [D, FF]), din("f1u", [D, FF]), din("f1d", [FF, D])
    f2g, f2u, f2d = din("f2g", [D, FF]), din("f2u", [D, FF]), din("f2d", [FF, D])
    w_in = din("w_in", [D, 2560])
    w_out = din("w_out", [D, D])
    wqkv = din("wqkv", [3, 4, 128, 128])
    w_if = din("w_if", [1536, 8])
    b_if = din("b_if", [8])
    y = nc.dram_tensor("y", [S_LEN, D], F32, kind="ExternalOutput").ap()

    es = ExitStack()
    with es:
        S = Sched(nc)

        def sb(name, shape, dt=F32):
            return es.enter_context(nc.sbuf_tensor(name, shape, dt))

        ident_f = sb("ident_f", [128, 128])
        ones_m = sb("ones_m", [128, 128], BF16)
        ones_f = sb("ones_f", [128, 128])
        tri_f = sb("tri_f", [128, 128])
        causal4 = sb("causal4", [128, 4, 512], BF16)
        ident_b = sb("ident_b", [128, 128], BF16)
        sel_f = sb("sel_f", [128, 128])
        lr_all = sb("lr_all", [128, 2, TT])
        E01 = sb("E01", [128, 32])
        ELB = sb("ELB", [128, 32])
        OWN = sb("OWN", [128, 32])
        vecT = sb("vecT", [128, 72])
        modT = sb("modT", [128, 72])
        prm = sb("prm", [128, 9, 8])
        modrow = sb("modrow", [1, 2, 256])
        wqkv_b = sb("wqkv_b", [128, 12, 128], BF16)
        wif_sb = sb("wif_sb", [128, 12, 8])
        wg_b = sb("wg_b", [128, 8, 8], BF16)
        bif_bc = sb("bif_bc", [128, 8])
        Kc = sb("Kc", [128, 4, S_LEN], BF16)
        Vc = sb("Vc", [128, NKT, 8, 65], BF16)
        km_f = sb("km_f", [128, 4, 16])
        km_hi = sb("km_hi", [128, 4, 16], BF16)
        km_lo = sb("km_lo", [128, 4, 16], BF16)
        Cst = sb("Cst", [128, 4, 129])
        Cb = sb("Cb", [128, 4, 129], BF16)
        xm_halo = sb("xm_halo", [128, 4, 3], BF16)
        xT = sb("xT", [128, 8, TT])
        hT = sb("hT", [128, 8, TT], BF16)
        tmpf = sb("tmpf", [128, 2, TT])
        rstd = sb("rstd", [128, 2, TT])
        ring = sb("ring", [128, NR * 2048], BF16)
        UB = 66 * 1024
        U = sb("U", [128, UB // 2], BF16)
        Uf = U.bitcast(F32)
        ringf = ring.bitcast(F32)
        stg = ringf[:, 0:1536].rearrange("p (a e) -> p a e", a=12)
        wqkvT = ringf[:, 1536:3072].rearrange("p (a e) -> p a e", a=12)
        PS = [es.enter_context(nc.psum_tensor(f"ps{i}", [128, 512], F32)) for i in range(8)]

        class Carver:
            def __init__(self):
                self.off = 0

            def b(self, n):
                o = self.off
                self.off += -(-n * 2 // 64) * 64
                assert self.off <= UB, self.off
                return U[:, o // 2:o // 2 + n]

            def f(self, n):
                o = self.off
                self.off += -(-n * 4 // 64) * 64
                assert self.off <= UB, self.off
                return Uf[:, o // 4:o // 4 + n]

        cv = Carver()
        act = cv.b(NFF * TT).rearrange("p (j t) -> p j t", j=NFF)
        x_tm = cv.f(4 * D).rearrange("p (s d) -> p s d", s=4)
        y_tm = cv.f(4 * D).rearrange("p (s d) -> p s d", s=4)
        cv_mark = cv.off
        cv.off = 0
        sq = cv.b(8 * TT).rearrange("p (c t) -> p c t", c=8)
        cv.off = 0
        PT = cv.b(3 * TT).rearrange("p (c t) -> p c t", c=3)
        biasT = cv.b(TT)
        xm_b = cv.b(4 * 515).rearrange("p (c t) -> p c t", c=4)
        Qz = cv.b(8 * TT).rearrange("p (c t) -> p c t", c=8)
        sz = cv.b(4 * TT).rearrange("p (c t) -> p c t", c=4)
        xc_b = cv.b(4 * TT).rearrange("p (c t) -> p c t", c=4)
        xcs = cv.b(4 * TT).rearrange("p (c t) -> p c t", c=4)
        biasq = cv.f(4 * 128).rearrange("p (s h k) -> p s h k", s=4, h=8)
        gm = cv.f(128).rearrange("p (h k) -> p h k", h=8)
        sel = cv.f(128).rearrange("p (h k) -> p h k", h=8)
        thr = cv.f(64).rearrange("p (h k) -> p h k", h=8)
        convacc = cv.f(TT)
        att_f = cv.f(4 * TT).rearrange("p (c t) -> p c t", c=4)
        o_sb = [cv.f(TT), cv.f(TT)]
        lr_f = [lr_all[:, 0, :], lr_all[:, 1, :]]
        QtS = [cv.b(512).rearrange("p (h t) -> p h t", h=4) for _ in range(2)]
        KtS = [cv.b(512).rearrange("p (h t) -> p h t", h=4)] * 2
        Ktok = [cv.b(512).rearrange("p (h t) -> p h t", h=4)] * 2
        Vp = [cv.b(4 * 129).rearrange("p (h t) -> p h t", h=4)] * 2
        Sc = [cv.b(512).rearrange("p (h t) -> p h t", h=4)] * 2
        hn = [cv.f(512).rearrange("p (h t) -> p h t", h=4)] * 2
        gsm = [cv.f(64), cv.f(64)]
        bst = cv.f(4 * 6).rearrange("p (h k) -> p h k", h=4)
        mv = cv.f(8).rearrange("p (h k) -> p h k", h=4)
        ctmp = cv.f(4 * 129).rearrange("p (h t) -> p h t", h=4)
        o_cat = cv.b(8 * TT).rearrange("p (c t) -> p c t", c=8)
        wa_st = x_tm.rearrange("p s (k n) -> p (s k) n", n=256)
        wa_bufs = [wa_st[:, 0:8, :], wa_st[:, 8:16, :]]

        def mm(out, lhsT, rhs, start, stop, inc=None):
            inc = stop if inc is None else inc
            S.op("pe", lambda e: e.matmul(out, lhsT=lhsT, rhs=rhs, start=start, stop=stop),
                 reads=[lhsT, rhs], writes=[out], inc=inc)

        def tr(out, in_, ident, inc=True):
            S.op("pe", lambda e: e.transpose(out=out, in_=in_, identity=ident),
                 reads=[in_, ident], writes=[out], inc=inc)

        def actf(out, in_, func, bias=0.0, scale=1.0, extra_reads=()):
            S.op("act", lambda e: e.activation(out=out, in_=in_, func=func, bias=bias, scale=scale),
                 reads=[in_] + list(extra_reads), writes=[out])

        def acopy(out, in_):
            S.op("act", lambda e: e.copy(out=out, in_=in_), reads=[in_], writes=[out])

        def vcopy(out, in_):
            S.op("dve", lambda e: e.tensor_copy(out=out, in_=in_), reads=[in_], writes=[out])

        def vtt(out, in0, in1, op):
            S.op("dve", lambda e: e.tensor_tensor(out=out, in0=in0, in1=in1, op=op), reads=[in0, in1], writes=[out])

        def vts(out, in0, s1, s2, op0, op1=None, rd=()):
            if op1 is None:
                S.op("dve", lambda e: e.tensor_scalar(out=out, in0=in0, scalar1=s1, scalar2=None, op0=op0),
                     reads=[in0] + list(rd), writes=[out])
            else:
                S.op("dve", lambda e: e.tensor_scalar(out=out, in0=in0, scalar1=s1, scalar2=s2, op0=op0, op1=op1),
                     reads=[in0] + list(rd), writes=[out])

        def vstt(out, in0, scalar, in1, op0, op1, rd=()):
            S.op("dve", lambda e: e.scalar_tensor_tensor(out=out, in0=in0, scalar=scalar, in1=in1, op0=op0, op1=op1),
                 reads=[in0, in1] + list(rd), writes=[out])

        def pmemset(ap, v):
            S.op("pool", lambda e: e.memset(ap, v), writes=[ap])

        def vmemset(ap, v):
            S.op("dve", lambda e: e.memset(ap, v), writes=[ap])

        def pasel(ap, pattern, cmp, cm, base=0):
            S.op("pool", lambda e: e.affine_select(out=ap, in_=ap, pattern=pattern, compare_op=cmp, fill=0.0,
                                                   base=base, channel_multiplier=cm), reads=[ap], writes=[ap])

        dumps = {}

        def chk(st):
            if stage <= st:
                raise _Stop()

        def dump(name, ap, dt=F32):
            if not dbg or name in dumps:
                return
            shp = list(ap.shape)
            dumps[name] = nc.dram_tensor("dbg_" + name, shp, dt, kind="ExternalOutput").ap()
            S.dma("sp", dumps[name], ap, "dbg_" + name)

        pmemset(ident_f[:], 1.0)
        pasel(ident_f[:], [[-1, 128]], ALU.is_equal, 1)
        pmemset(ones_m[:], 1.0 / D)
        pmemset(ones_f[:], 1.0)
        pmemset(tri_f[:], 1.0)
        pasel(tri_f[:], [[1, 128]], ALU.is_ge, -1)
        pmemset(causal4[:], 1.0)
        pasel(causal4[:], [[-128, 4], [1, 512]], ALU.is_ge, -1)
        pmemset(ident_b[:], 1.0)
        pasel(ident_b[:], [[-1, 128]], ALU.is_equal, 1)
        pmemset(sel_f[:], 1.0)
        pasel(sel_f[:], [[0, 128]], ALU.is_equal, 1, base=-64)
        pmemset(lr_all[:], 0.0)
        pmemset(E01[:, 0:16], 1.0)
        pmemset(E01[:, 16:32], 0.0)
        pmemset(ELB[:, 0:16], 0.0)
        pmemset(ELB[:, 16:32], -1e30)
        pmemset(OWN[:], 0.0)
        pmemset(OWN[:, 16:17], 1.0)
        pmemset(Vc[:, :, :, 64:65], 1.0)
        pmemset(km_f[:], 0.0)
        pmemset(km_hi[:], 0.0)
        pmemset(km_lo[:], 0.0)
        pmemset(Cst[:], 0.0)
        pmemset(Cb[:], 0.0)
        pmemset(xm_halo[:], 0.0)

        S.dma("sp", stg[0:72, 0, :], vecs, "ldv1")
        tr(PS[0][:, 0:72], stg[0:72, 0, :], ident_f[0:72, 0:72])
        vcopy(vecT[:], PS[0][:, 0:72])
        S.dma("sp", stg[0:72, 1, :], bada, "ldv2")
        tr(PS[1][:, 0:72], stg[0:72, 1, :], ident_f[0:72, 0:72])
        vcopy(modT[:], PS[1][:, 0:72])
        S.dma("sp", bif_bc[:], b_if.partition_broadcast(128), "ldv3")
        S.dma("sp", wif_sb[:], w_if.rearrange("(j p) g -> p j g", p=128), "ldv4")

        NG = 36
        for g in range(NG):
            buf = wa_bufs[g % 2]
            S.dma("sp", buf, w_ada[:, g * 256:(g + 1) * 256].rearrange("(kc p) n -> p kc n", p=128), f"lda{g % 2}")
            pso = PS[2 + g % 2]
            for kc in range(8):
                mm(pso[0:1, 0:256], vecT[:, kc:kc + 1], buf[:, kc, :], kc == 0, kc == 7)
            acopy(modrow[0:1, g % 2, :], pso[0:1, 0:256])
            for jj in range(2):
                j = 2 * g + jj
                mm(PS[4][:, j:j + 1], modrow[0:1, g % 2, jj * 128:(jj + 1) * 128], ones_f[0:1, 0:1], True, True)
        vtt(modT[:], modT[:], PS[4][:, 0:72], ALU.add)

        def modv(i):
            return modT[:, i * 8:(i + 1) * 8]

        def vrow(r, n=8):
            return vecT[:, r:r + n]
        for blk, (nrow, half) in enumerate(((8, 0.5), (16, 1.0), (24, 0.5))):
            vstt(prm[:, 3 * blk + 0, :], modv(3 * blk + 1), 1.0, vrow(nrow), ALU.add, ALU.mult)
            vcopy(prm[:, 3 * blk + 1, :], modv(3 * blk + 0))
            vts(prm[:, 3 * blk + 2, :], modv(3 * blk + 2), 1.0, half, ALU.add, ALU.mult)
        fin_g = vrow(32)
        attn_g = vrow(40, 4)
        mn_g = vrow(44, 4)
        skip_g = vrow(48, 4)
        conv_bv = vrow(52, 4)

        def conv_wv(j, hc):
            return vecT[:, 56 + j * 4 + hc:57 + j * 4 + hc]

        S.dma("sp", stg[:], wqkv.rearrange("a h d e -> d (a h) e"), "ldv5")
        vcopy(wqkv_b[:], stg[:])
        for i in range(12):
            tr(PS[5 + i % 2][:, 0:128], stg[:, i, :], ident_f[:])
            acopy(wqkvT[:, i, :], PS[5 + i % 2][:, 0:128])
        for h in range(4):
            mm(PS[7][:, h * 8:(h + 1) * 8], wqkvT[:, h, :], wif_sb[:, 3 * h, :], True, False)
            mm(PS[7][:, h * 8:(h + 1) * 8], wqkvT[:, 4 + h, :], wif_sb[:, 3 * h + 1, :], False, True, inc=False)
            mm(PS[7][:, (4 + h) * 8:(5 + h) * 8], wqkvT[:, 8 + h, :], wif_sb[:, 3 * h + 2, :], True, True, inc=(h == 3))
        vcopy(wg_b[:].rearrange("p a g -> p (a g)"), PS[7][:, 0:64])

        units = []

        def add_unit_cols(w, c0):
            units.append(w[:, c0:c0 + 256].rearrange("(kc p) n -> p kc n", p=128))

        def add_unit_rows(w, r0):
            units.append(w[r0:r0 + 256, :].rearrange("(jj p) n -> p jj n", p=128))

        for t in range(NT):
            for g in range(11):
                add_unit_cols(f1g, g * 256)
                add_unit_cols(f1u, g * 256)
            for g in range(11):
                add_unit_rows(f1d, g * 256)
            for g in range(10):
                add_unit_cols(w_in, g * 256)
            for g in range(4):
                add_unit_cols(w_out, g * 256)
            for g in range(11):
                add_unit_cols(f2g, g * 256)
                add_unit_cols(f2u, g * 256)
            for g in range(11):
                add_unit_rows(f2d, g * 256)
        wstate = {"issued": 0, "next": 0}

        def wview(u, rows):
            slot = ring[:, (u % NR) * 2048:(u % NR + 1) * 2048]
            if rows:
                return slot.rearrange("p (jj n) -> p jj n", jj=2)
            return slot.rearrange("p (kc n) -> p kc n", kc=8)

        def wissue_upto(n):
            while wstate["issued"] < min(n, len(units)):
                u = wstate["issued"]
                src = units[u]
                rows = (src.shape[1] == 2)
                S.dma("pool", wview(u, rows), src, f"w{u % NR}")
                wstate["issued"] += 1

        def wnext(rows=False):
            u = wstate["next"]
            wstate["next"] += 1
            wissue_upto(u + NR - 1)
            return wview(u, rows)

        def rms_mod(ns, sh):
            for c in range(8):
                actf(sq[:, c, :], xT[:, c, :], AF.Square)
            for c in range(8):
                mm(PS[6][:], ones_m[:], sq[:, c, :], c == 0, c == 7)
            actf(rstd[:, 0, :], PS[6][:], AF.Sqrt, bias=eps_t[:, 0:1], scale=1.0, extra_reads=[eps_t[:, 0:1]])
            S.op("dve", lambda e: e.reciprocal(out=rstd[:, 1, :], in_=rstd[:, 0, :]),
                 reads=[rstd[:, 0, :]], writes=[rstd[:, 1, :]])
            for c in range(8):
                vstt(tmpf[:, c % 2, :], xT[:, c, :], ns[:, c:c + 1], rstd[:, 1, :], ALU.mult, ALU.mult,
                     rd=[ns[:, c:c + 1]])
                actf(hT[:, c, :], tmpf[:, c % 2, :], AF.Identity, bias=sh[:, c:c + 1], scale=1.0,
                     extra_reads=[sh[:, c:c + 1]])

        def ffn(rg):
            for g in range(11):
                wg_v = wnext()
                wu_v = wnext()
                for jj in range(2):
                    j = 2 * g + jj
                    pg, pu = PS[j % 2], PS[2 + j % 2]
                    for kc in range(8):
                        mm(pg[:], wg_v[:, kc, jj * 128:(jj + 1) * 128], hT[:, kc, :], kc == 0, kc == 7)
                    for kc in range(8):
                        mm(pu[:], wu_v[:, kc, jj * 128:(jj + 1) * 128], hT[:, kc, :], kc == 0, kc == 7)
                    actf(tmpf[:, j % 2, :], pg[:], AF.Silu)
                    vtt(act[:, j, :], tmpf[:, j % 2, :], pu[:], ALU.mult)
            dump('act', act[:], BF16)
            for g in range(10):
                wd_v = wnext(rows=True)
                for jj in range(2):
                    j = 2 * g + jj
                    for i in range(8):
                        mm(PS[i][:], wd_v[:, jj, i * 128:(i + 1) * 128], act[:, j, :], j == 0, False,
                           inc=(i == 7 and jj == 1))
            wd_v = wnext(rows=True)
            for i in range(8):
                for jj in range(2):
                    j = 20 + jj
                    mm(PS[i][:], wd_v[:, jj, i * 128:(i + 1) * 128], act[:, j, :], False, jj == 1, inc=(jj == 1))
                vstt(xT[:, i, :], PS[i][:], rg[:, i:i + 1], xT[:, i, :], ALU.mult, ALU.add, rd=[rg[:, i:i + 1]])

        eps_t = sb("eps_t", [128, 1])
        pmemset(eps_t[:], EPS)

        def mixer(t):
            c0 = t * TT
            for hh in range(8):
                z0 = 64 * (1 - hh % 2)
                vmemset(Qz[z0:z0 + 64, hh, :], 0.0)
            for g in range(4):
                wv_ = wnext()
                for jj in range(2):
                    oc = 2 * g + jj
                    ps = PS[4 + oc % 2]
                    for kc in range(8):
                        mm(ps[:], wv_[:, kc, jj * 128:(jj + 1) * 128], hT[:, kc, :], kc == 0, kc == 7)
                    if oc < 4:
                        acopy(Qz[0:64, 2 * oc, :], ps[0:64, :])
                        acopy(Qz[64:128, 2 * oc + 1, :], ps[64:128, :])
                    else:
                        vcopy(Kc[:, oc - 4, c0:c0 + TT], ps[:])
            for g in range(2):
                wv_ = wnext()
                for s4 in range(4):
                    ps = PS[6 + s4 % 2]
                    for kc in range(8):
                        mm(ps[:, 0:256], hT[:, kc, s4 * 128:(s4 + 1) * 128], wv_[:, kc, :], kc == 0, kc == 7)
                    dst = Vc[:, t * 4 + s4, g * 4:(g + 1) * 4, 0:64]
                    src = ps[:, 0:256].rearrange("p (h e) -> p h e", h=4)
                    if s4 % 2 == 0:
                        acopy(dst, src)
                    else:
                        vcopy(dst, src)
            vcopy(xm_b[:, :, 0:3], xm_halo[:])
            for g in range(2):
                wv_ = wnext()
                for jj in range(2):
                    oc = 2 * g + jj
                    ps = PS[4 + oc % 2]
                    for kc in range(8):
                        mm(ps[:], wv_[:, kc, jj * 128:(jj + 1) * 128], hT[:, kc, :], kc == 0, kc == 7)
                    acopy(xm_b[:, oc, 3:515], ps[:])
            vcopy(xm_halo[:], xm_b[:, :, 512:515])
            for g in range(2):
                wv_ = wnext()
                for jj in range(2):
                    oc = 2 * g + jj
                    ps = PS[6 + oc % 2]
                    for kc in range(8):
                        mm(ps[:], wv_[:, kc, jj * 128:(jj + 1) * 128], hT[:, kc, :], kc == 0, kc == 7)
                    actf(sz[:, oc, :], ps[:], AF.Silu)
            for hc in range(4):
                vts(convacc, xm_b[:, hc, 0:512], conv_wv(0, hc), conv_bv[:, hc:hc + 1], ALU.mult, ALU.add,
                    rd=[conv_wv(0, hc), conv_bv[:, hc:hc + 1]])
                for j in range(1, 4):
                    vstt(convacc, xm_b[:, hc, j:j + 512], conv_wv(j, hc), convacc, ALU.mult, ALU.add,
                         rd=[conv_wv(j, hc)])
                actf(xc_b[:, hc, :], convacc, AF.Silu)
                vts(xcs[:, hc, :], xc_b[:, hc, :], skip_g[:, hc:hc + 1], None, ALU.mult, rd=[skip_g[:, hc:hc + 1]])
            dump('xm_b', xm_b[:], BF16); dump('sz', sz[:], BF16); dump('xc_b', xc_b[:], BF16)
            chk(3)
            attention(t, None)
            chk(4)
            dump('att_f', att_f[:]); dump('biasq', biasq[:])
            gens = [mlstm_chunk(ch, PS[2 * (ch % 2)], PS[2 * (ch % 2) + 1]) for ch in range(4)]
            LAG = 7
            active, nstep, nxt = [], {}, 0
            while active or nxt < 4:
                if nxt < 4 and (not active or nstep[active[-1]] >= LAG) and len(active) < 2:
                    active.append(nxt)
                    nstep[nxt] = 0
                    nxt += 1
                for g in list(active):
                    try:
                        next(gens[g])
                        nstep[g] += 1
                    except StopIteration:
                        active.remove(g)
            chk(5)
            dump('o_cat', o_cat[:], BF16)
            rg = prm[:, 5, :]
            for g in range(4):
                wv_ = wnext()
                for jj in range(2):
                    oc = 2 * g + jj
                    ps = PS[4 + oc % 2]
                    for kc in range(8):
                        mm(ps[:], wv_[:, kc, jj * 128:(jj + 1) * 128], o_cat[:, kc, :], kc == 0, kc == 7)
                    vstt(xT[:, oc, :], ps[:], rg[:, oc:oc + 1], xT[:, oc, :], ALU.mult, ALU.add, rd=[rg[:, oc:oc + 1]])

        def attention(t, mgen=None):
            c0 = t * TT
            for c in range(4):
                for b in range(2):
                    blk = 2 * t + b
                    src = Kc[:, c, blk * 256:(blk + 1) * 256]
                    S.op("dve", lambda e, src=src, c=c, blk=blk: e.tensor_reduce(
                        out=km_f[:, c, blk:blk + 1], in_=src, axis=AX.X, op=ALU.add),
                        reads=[src], writes=[km_f[:, c, blk:blk + 1]])
                kk = km_f[:, c, 2 * t:2 * t + 2]
                vts(kk, kk, 1.0 / 256, None, ALU.mult)
                vcopy(km_hi[:, c, 2 * t:2 * t + 2], kk)
                vtt(kk, kk, km_hi[:, c, 2 * t:2 * t + 2], ALU.subtract)
                vcopy(km_lo[:, c, 2 * t:2 * t + 2], kk)
            chk(3.1)
            for s4 in range(4):
                qb = 2 * t + s4 // 2
                for h in range(8):
                    c, r0 = h // 2, 64 * (h % 2)
                    psG = PS[6 + h % 2]
                    qsl = Qz[:, h, s4 * 128:(s4 + 1) * 128]
                    mm(psG[:, c * 16:(c + 1) * 16], qsl, km_hi[:, c, :], True, False)
                    mm(psG[:, c * 16:(c + 1) * 16], qsl, km_lo[:, c, :], False, True, inc=(h >= 6))
                chk(3.11)
                elb = ELB[:, 16 - qb:32 - qb].unsqueeze(1).broadcast_to([128, 8, 16])
                e01 = E01[:, 16 - qb:32 - qb].unsqueeze(1).broadcast_to([128, 8, 16])
                own = OWN[:, 16 - qb:32 - qb].unsqueeze(1).broadcast_to([128, 8, 16])
                gm4 = gm.rearrange("p (c two) k -> p c two k", two=2)
                for par in range(2):
                    vtt(gm4[:, :, par, :], PS[6 + par][:, 0:64].rearrange("p (c k) -> p c k", c=4),
                        ELB[:, 16 - qb:32 - qb].unsqueeze(1).broadcast_to([128, 4, 16]), ALU.add)
                chk(3.12)
                for h in range(8):
                    S.op("dve", lambda e, h=h: e.max(out=thr[:, h, :], in_=gm[:, h, :]),
                         reads=[gm[:, h, :]], writes=[thr[:, h, :]])
                chk(3.13)
                vtt(sel, gm, thr[:, :, 2:3].broadcast_to([128, 8, 16]), ALU.is_ge)
                chk(3.14)
                vtt(sel, sel, e01, ALU.mult)
                vtt(sel, sel, own, ALU.add)
                vts(biasq[:, s4, :, :], sel, 1.0, BIG, ALU.subtract, ALU.mult)
                chk(3.15)
                if s4 == 1:
                    chk(3.16)
            chk(3.2)
            for s4 in range(4):
                tr(PS[7][:, s4 * 128:(s4 + 1) * 128], biasq[:, s4, :, :].rearrange("p h k -> p (h k)"), ident_f[:],
                   inc=(s4 == 3))
            vcopy(biasT, PS[7][:])
            nkt = 4 * (t + 1)
            LA = 2
            pend = None
            for h in range(8):
                c, r0 = h // 2, 64 * (h % 2)
                pso = PS[3 + h % 2]

                def qk(kt, c=c, h=h):
                    pss = PS[kt % 3]
                    j = 16 * h + kt // 2
                    mm(pss[:], Kc[:, c, kt * 128:(kt + 1) * 128], Qz[:, h, :], True, False)
                    mm(pss[:], ident_b[:, j:j + 1].broadcast_to([128, 128]), biasT, False, True)

                for kt in range(min(LA, nkt)):
                    qk(kt)
                if pend is not None:
                    pend()
                    pend = None
                for kt in range(nkt):
                    if kt + LA < nkt:
                        qk(kt + LA)
                    pss = PS[kt % 3]
                    pt = PT[:, kt % 3, :]
                    actf(pt, pss[:], AF.Exp, scale=0.125)
                    if kt >= 4 * t:
                        vtt(pt, pt, causal4[:, kt - 4 * t, :], ALU.mult)
                    mm(pso[0:65, :], Vc[:, kt, h, :], pt, kt == 0, kt == nkt - 1, inc=True)
                    if mgen is not None:
                        next(mgen, None)
                lr, osb, psb = lr_f[h % 2], o_sb[h % 2], PS[5 + h % 2]
                actf(lr[32:33, :], pso[64:65, :], AF.Ln)
                actf(lr[64:65, :], lr[32:33, :], AF.Exp, scale=-1.0)
                acopy(osb[0:64, :], pso[0:64, :])

                def ep(lr=lr, osb=osb, psb=psb, c=c, r0=r0):
                    mm(psb[:], sel_f[:], lr, True, True)
                    vtt(att_f[r0:r0 + 64, c, :], osb[0:64, :], psb[0:64, :], ALU.mult)
                pend = ep
            pend()
            chk(3.6)
            for c in range(4):
                actf(sq[:, c, :], att_f[:, c, :], AF.Square, scale=2.0 ** 0.5)
            for c in range(4):
                mm(PS[5][:], ones_m[:], sq[:, c, :], c == 0, c == 3)
            actf(rstd[:, 0, :], PS[5][:], AF.Sqrt, bias=eps_t[:, 0:1], scale=1.0, extra_reads=[eps_t[:, 0:1]])
            S.op("dve", lambda e: e.reciprocal(out=rstd[:, 1, :], in_=rstd[:, 0, :]),
                 reads=[rstd[:, 0, :]], writes=[rstd[:, 1, :]])
            for c in range(4):
                vstt(o_cat[:, c, :], att_f[:, c, :], attn_g[:, c:c + 1], rstd[:, 1, :], ALU.mult, ALU.mult,
                     rd=[attn_g[:, c:c + 1]])

        def mlstm_chunk(ch, A, Bk):
            kscale = 128.0 ** -0.5
            if True:
                cs = slice(ch * 128, (ch + 1) * 128)
                cs3 = slice(3 + ch * 128, 3 + (ch + 1) * 128)
                b2 = ch % 2
                gs = gsm[b2]
                for h in range(4):
                    mm(A[:, 0:8], xc_b[:, h, cs], wg_b[:, h, :], h == 0, False)
                for h in range(4):
                    mm(A[:, 0:8], xm_b[:, h, cs3], wg_b[:, 4 + h, :], False, h == 3)
                vtt(gs[:, 0:8], A[:, 0:8], bif_bc[:], ALU.add)
                actf(gs[:, 28:32], gs[:, 4:8], AF.Exp, scale=-1.0)
                actf(gs[:, 4:8], gs[:, 28:32], AF.Ln, bias=1.0)
                vts(gs[:, 4:8], gs[:, 4:8], -1.0, None, ALU.mult)
                yield
                yield
                mm(A[:, 16:20], tri_f[:], gs[:, 4:8], True, True)
                mm(A[:, 24:28], ones_f[:], gs[:, 4:8], True, True)
                vcopy(gs[:, 8:16].rearrange("p (a k) -> p a k", a=2),
                      A[:, 16:32].rearrange("p (a k) -> p a k", a=2)[:, :, 0:4])
                vtt(gs[:, 28:32], gs[:, 0:4], gs[:, 8:12], ALU.subtract)
                actf(gs[:, 16:20], gs[:, 28:32], AF.Exp)
                actf(gs[:, 20:28], gs[:, 8:16], AF.Exp)
                yield
                for h in range(4):
                    mm(Bk[:, h * 128:(h + 1) * 128], wqkv_b[:, h, :], xc_b[:, h, cs], True, True, inc=(h == 3))
                vcopy(QtS[b2].rearrange("p h t -> p (h t)"), Bk[:])
                yield
                for h in range(4):
                    mm(A[:, h * 128:(h + 1) * 128], wqkv_b[:, 4 + h, :], xc_b[:, h, cs], True, True, inc=(h == 3))
                vts(KtS[b2].rearrange("p h t -> p (h t)"), A[:], kscale, None, ALU.mult)
                yield
                for h in range(4):
                    mm(Bk[:, h * 128:(h + 1) * 128], xc_b[:, h, cs], wqkv_b[:, 4 + h, :], True, True, inc=(h == 3))
                vts(Ktok[b2].rearrange("p h t -> p (h t)"), Bk[:], kscale, None, ALU.mult)
                yield
                for h in range(4):
                    mm(A[:, h * 128:(h + 1) * 128], xm_b[:, h, cs3], wqkv_b[:, 8 + h, :], True, True, inc=(h == 3))
                a_bc = gs[:, 16:20].unsqueeze(2).broadcast_to([128, 4, 128])
                vtt(Vp[b2][:, :, 0:128], A[:].rearrange("p (h e) -> p h e", h=4), a_bc, ALU.mult)
                vcopy(Vp[b2][:, :, 128:129], gs[:, 16:20].unsqueeze(2))
                yield
                for h in range(4):
                    mm(Bk[:, h * 128:(h + 1) * 128], KtS[b2][:, h, :], QtS[b2][:, h, :], True, True, inc=(h == 3))
                vtt(Sc[b2][:], Bk[:].rearrange("p (h t) -> p h t", h=4),
                    tri_f[:].unsqueeze(1).broadcast_to([128, 4, 128]), ALU.mult)
                yield
                pU = [A, Bk]
                for h in range(4):
                    o = pU[h // 2][:, (h % 2) * 129:(h % 2) * 129 + 129]
                    mm(o, Ktok[b2][:, h, :], Vp[b2][:, h, :], True, True, inc=(h % 2 == 1))
                for hp in range(2):
                    vtt(ctmp[:, 2 * hp:2 * hp + 2, :], Cst[:, 2 * hp:2 * hp + 2, :],
                        pU[hp][:, 0:258].rearrange("p (h v) -> p h v", h=2), ALU.add)
                yield
                pH = [A, Bk]
                for h in range(4):
                    o = pH[h // 2][:, (h % 2) * 129:(h % 2) * 129 + 129]
                    mm(o, Sc[b2][:, h, :], Vp[b2][:, h, :], True, False)
                    mm(o, QtS[b2][:, h, :], Cb[:, h, :], False, True, inc=(h % 2 == 1))
                vtt(Cst[:], ctmp[:], gs[:, 24:28].unsqueeze(2).broadcast_to([128, 4, 129]), ALU.mult)
                vcopy(Cb[:], Cst[:])
                for h in range(4):
                    hp, hh = h // 2, h % 2
                    S.op("dve", lambda e, hp=hp, hh=hh, h=h: e.bn_stats(out=bst[:, h, :], in_=pH[hp][:, hh * 129:hh * 129 + 128]),
                         reads=[pH[hp][:, hh * 129:hh * 129 + 128]], writes=[bst[:, h, :]])
                for hp in range(2):
                    Hv = pH[hp][:, 0:258].rearrange("p (h v) -> p h v", h=2)
                    vtt(gs[:, 28 + 2 * hp:30 + 2 * hp].unsqueeze(2), Hv[:, :, 128:129],
                        gs[:, 20 + 2 * hp:22 + 2 * hp].unsqueeze(2), ALU.mult)
                for h in range(4):
                    S.op("dve", lambda e, h=h: e.bn_aggr(out=mv[:, h, :], in_=bst[:, h, :]),
                         reads=[bst[:, h, :]], writes=[mv[:, h, :]])
                vts(gs[:, 52:56], gs[:, 28:32], -1.0, None, ALU.mult)
                vtt(gs[:, 32:36], gs[:, 28:32], gs[:, 52:56], ALU.max)
                vts(gs[:, 32:36], gs[:, 32:36], 1.0, None, ALU.max)
                S.op("dve", lambda e, gs=gs: e.reciprocal(out=gs[:, 36:40], in_=gs[:, 32:36]),
                     reads=[gs[:, 32:36]], writes=[gs[:, 36:40]])
                vtt(gs[:, 36:40], gs[:, 36:40], gs[:, 20:24], ALU.mult)
                vtt(gs[:, 40:44], gs[:, 36:40], gs[:, 36:40], ALU.mult)
                vtt(gs[:, 40:44].unsqueeze(2), gs[:, 40:44].unsqueeze(2), mv[:, :, 1:2], ALU.mult)
                actf(gs[:, 44:48], gs[:, 40:44], AF.Sqrt, bias=eps_t[:, 0:1], scale=1.0, extra_reads=[eps_t[:, 0:1]])
                S.op("dve", lambda e, gs=gs: e.reciprocal(out=gs[:, 48:52], in_=gs[:, 44:48]),
                     reads=[gs[:, 44:48]], writes=[gs[:, 48:52]])
                vtt(gs[:, 48:52], gs[:, 48:52], gs[:, 36:40], ALU.mult)
                for h in range(4):
                    hp, hh = h // 2, h % 2
                    vts(hn[b2][:, h, :], pH[hp][:, hh * 129:hh * 129 + 128], mv[:, h, 0:1], gs[:, 48 + h:49 + h],
                        ALU.subtract, ALU.mult, rd=[mv[:, h, 0:1], gs[:, 48 + h:49 + h]])
                yield
                yield
                yield
                for h in range(4):
                    tr(A[:, h * 128:(h + 1) * 128], hn[b2][:, h, :], ident_f[:], inc=(h == 3))
                for h in range(4):
                    vstt(tmpf[:, 0, h * 128:(h + 1) * 128], A[:, h * 128:(h + 1) * 128], mn_g[:, h:h + 1],
                         xcs[:, h, cs], ALU.mult, ALU.add, rd=[mn_g[:, h:h + 1]])
                    vtt(o_cat[:, 4 + h, cs], tmpf[:, 0, h * 128:(h + 1) * 128], sz[:, h, cs], ALU.mult)
                yield

        try:
          chk(0)
          for t in range(NT):
              c0 = t * TT
              if t == 0:
                  S.dma("sp", x_tm, x[0:TT, :].rearrange("(s p) d -> p s d", p=128), "ldx")
              for c in range(8):
                  ps = PS[6 + c % 2]
                  for s4 in range(4):
                      tr(ps[:, s4 * 128:(s4 + 1) * 128], x_tm[:, s4, c * 128:(c + 1) * 128], ident_f[:], inc=(s4 == 3))
                  acopy(xT[:, c, :], ps[:])
              dump('modT', modT[:]); dump('prm', prm[:]); dump('xT0', xT[:])
              rms_mod(prm[:, 0, :], prm[:, 1, :])
              dump('hT1', hT[:], BF16)
              chk(1)
              ffn(prm[:, 2, :])
              chk(2)
              dump('xT1', xT[:])
              rms_mod(prm[:, 3, :], prm[:, 4, :])
              dump('hT2', hT[:], BF16)
              mixer(t)
              if t + 1 < NT:
                  S.dma("sp", x_tm, x[c0 + TT:c0 + 2 * TT, :].rearrange("(s p) d -> p s d", p=128), "ldx")
              dump('xT2', xT[:])
              rms_mod(prm[:, 6, :], prm[:, 7, :])
              ffn(prm[:, 8, :])
              for c in range(8):
                  actf(sq[:, c, :], xT[:, c, :], AF.Square)
              for c in range(8):
                  mm(PS[6][:], ones_m[:], sq[:, c, :], c == 0, c == 7)
              actf(rstd[:, 0, :], PS[6][:], AF.Sqrt, bias=eps_t[:, 0:1], scale=1.0, extra_reads=[eps_t[:, 0:1]])
              S.op("dve", lambda e: e.reciprocal(out=rstd[:, 1, :], in_=rstd[:, 0, :]),
                   reads=[rstd[:, 0, :]], writes=[rstd[:, 1, :]])
              for c in range(8):
                  vstt(tmpf[:, c % 2, :], xT[:, c, :], fin_g[:, c:c + 1], rstd[:, 1, :], ALU.mult, ALU.mult,
                       rd=[fin_g[:, c:c + 1]])
                  ps = PS[4 + c % 2]
                  for s4 in range(4):
                      tr(ps[:, s4 * 128:(s4 + 1) * 128], tmpf[:, c % 2, s4 * 128:(s4 + 1) * 128], ident_f[:], inc=(s4 == 3))
                  acopy(y_tm[:, :, c * 128:(c + 1) * 128], ps[:].rearrange("p (s d) -> p s d", s=4))
              S.dma("sp", y[c0:c0 + TT, :].rearrange("(s p) d -> p s d", p=128), y_tm, "sty")
        except _Stop:
            pass
        S.final_wait("sp")
        S.emit(es)
    return nc


def make_in_maps(S_LEN, x, c, w_ada, b_ada, ffn1_norm, ffn1_w_gate, ffn1_w_up, ffn1_w_down,
                 mix_norm, w_in, conv_w, conv_b, w_q_m, w_k_m, w_v_m, w_if, b_if,
                 mlstm_norm, mlstm_skip, attn_norm, w_out,
                 ffn2_norm, ffn2_w_gate, ffn2_w_up, ffn2_w_down, final_norm):
    f = lambda a: np.ascontiguousarray(np.asarray(a, dtype=np.float32))
    B = x.shape[0]
    shared = {
        "bada": f(b_ada[0]).reshape(72, 128),
        "w_ada": f(w_ada[0]),
        "f1g": f(ffn1_w_gate[0]), "f1u": f(ffn1_w_up[0]), "f1d": f(ffn1_w_down[0]),
        "f2g": f(ffn2_w_gate[0]), "f2u": f(ffn2_w_up[0]), "f2d": f(ffn2_w_down[0]),
        "w_in": f(w_in[0]), "w_out": f(w_out[0]),
        "wqkv": f(np.stack([w_q_m[0], w_k_m[0], w_v_m[0]])),
        "w_if": f(w_if[0]), "b_if": f(b_if[0]),
    }
    maps = []
    for b in range(B):
        rows = [f(c[b]).reshape(8, 128), f(ffn1_norm[0]).reshape(8, 128), f(mix_norm[0]).reshape(8, 128),
                f(ffn2_norm[0]).reshape(8, 128), f(final_norm).reshape(8, 128), f(attn_norm[0]).reshape(4, 128),
                f(mlstm_norm[0]).reshape(4, 128), f(mlstm_skip[0]).reshape(4, 128), f(conv_b[0]).reshape(4, 128),
                f(conv_w[0]).reshape(16, 128)]
        m = dict(shared)
        m["vecs"] = np.ascontiguousarray(np.concatenate(rows, axis=0))
        m["x"] = f(x[b])
        maps.append(m)
    return maps


def kernel(**inputs):
    x = np.asarray(inputs["x"])
    B, S_LEN, _ = x.shape
    nc = build(S_LEN)
    maps = make_in_maps(S_LEN, **inputs)
    res = run_bass_kernel_spmd(nc, maps, core_ids=list(range(B)))
    return np.stack([np.asarray(r["y"], dtype=np.float32) for r in res.results], axis=0)
```
